# Optimizing a Trainium2 kernel written in Bass

```python
import jax, jax.numpy as jnp
from jax import lax
import numpy as np

D_MODEL = 2048
BATCH = 4
SEQ = 2048
DEPTH = 1

CHUNK = 128
SGU_GROUPS = 8
SGU_GROUP_DIM = D_MODEL // 16
SGU_WIDTH = SGU_GROUPS * SGU_GROUP_DIM
FOX_HEADS = 8
HEAD_DIM = 128
FOX_WIDTH = FOX_HEADS * HEAD_DIM
Q_BLOCK = 128
N_GROUPS = 4
EXPERTS_PER_GROUP = 8
N_EXPERTS = N_GROUPS * EXPERTS_PER_GROUP
TOP_K = 2
D_EXPERT = D_MODEL // 4
EXPERT_BLOCK = 128
EPS = 1e-6

OFF_U = 0
OFF_V = OFF_U + SGU_WIDTH
OFF_Q = OFF_V + SGU_WIDTH
OFF_K = OFF_Q + FOX_WIDTH
OFF_VA = OFF_K + FOX_WIDTH
OFF_F = OFF_VA + FOX_WIDTH
OFF_GATE = OFF_F + FOX_HEADS
IN_COLS = OFF_GATE + 2 * D_MODEL

kernel_name = 'hybrid_sgu_fox_hmoe_block'


def rms_norm(x, g):
    xf = x.astype(jnp.float32)
    y = xf * lax.rsqrt(jnp.mean(xf * xf, axis=-1, keepdims=True) + EPS)
    return (y * g.astype(jnp.float32)).astype(x.dtype)


def layer_norm(x, g, b):
    xf = x.astype(jnp.float32)
    mu = jnp.mean(xf, axis=-1, keepdims=True)
    var = jnp.mean(jnp.square(xf - mu), axis=-1, keepdims=True)
    y = (xf - mu) * lax.rsqrt(var + EPS)
    return (y * g.astype(jnp.float32) + b.astype(jnp.float32)).astype(x.dtype)


def chunked_sgu(u, v, ln_g, ln_b, w_s, b_s):
    B, S, _ = u.shape
    nc = S // CHUNK
    v = layer_norm(v, ln_g, ln_b)
    vg = v.reshape(B, nc, CHUNK, SGU_GROUPS, SGU_GROUP_DIM)
    causal = jnp.tril(jnp.ones((CHUNK, CHUNK), dtype=bool))
    w = jnp.where(causal[None], w_s, jnp.zeros_like(w_s))
    s = jnp.einsum('gts,bcsgd->bctgd', w, vg) + b_s.T[None, None, :, :, None]
    return u * s.reshape(B, S, SGU_WIDTH)


def forgetting_attention(q, k, v, f_logit, q_g, k_g):
    B, S, H, Dh = q.shape
    q = rms_norm(q, q_g)
    k = rms_norm(k, k_g)
    log_f = jax.nn.log_sigmoid(f_logit.astype(jnp.float32))
    c = jnp.cumsum(log_f, axis=1)
    c_k = c.transpose(0, 2, 1)
    nb = S // Q_BLOCK
    qb = q.reshape(B, nb, Q_BLOCK, H, Dh).transpose(1, 0, 2, 3, 4)
    cqb = c.reshape(B, nb, Q_BLOCK, H).transpose(1, 0, 3, 2)
    kpos = jnp.arange(S)
    scale = HEAD_DIM ** -0.5

    def block(args):
        qi, cqi, i = args
        s = jnp.einsum('bqhd,bkhd->bhqk', qi, k).astype(jnp.float32) * scale
        s = s + cqi[..., :, None] - c_k[:, :, None, :]
        qpos = i * Q_BLOCK + jnp.arange(Q_BLOCK)
        s = jnp.where(qpos[:, None] >= kpos[None, :], s, -jnp.inf)
        p = jax.nn.softmax(s, axis=-1)
        return jnp.einsum('bhqk,bkhd->bqhd', p.astype(v.dtype), v)

    o = lax.map(block, (qb, cqb, jnp.arange(nb)))
    return o.transpose(1, 0, 2, 3, 4).reshape(B, S, H * Dh)


def hierarchical_moe(h, w_rg, b_rg, w_re, b_re, w_g, w_u, w_d):
    B, S, D = h.shape
    T = B * S
    xt = h.reshape(T, D)
    g_logits = (xt @ w_rg + b_rg).astype(jnp.float32)
    g_prob = jax.nn.softmax(g_logits, axis=-1)
    grp = jnp.argmax(g_logits, axis=-1)
    p_grp = jnp.take_along_axis(g_prob, grp[:, None], axis=1)
    e_logits = (xt @ w_re + b_re).astype(jnp.float32).reshape(T, N_GROUPS, EXPERTS_PER_GROUP)
    e_logits = jnp.take_along_axis(e_logits, grp[:, None, None], axis=1)[:, 0]
    top_v, top_i = lax.top_k(e_logits, TOP_K)
    w_top = jax.nn.softmax(top_v, axis=-1) * p_grp
    eid = grp[:, None] * EXPERTS_PER_GROUP + top_i
    A = T * TOP_K
    eid_f = eid.reshape(A)
    tok_f = jnp.repeat(jnp.arange(T, dtype=jnp.int32), TOP_K)
    gate_f = w_top.reshape(A)
    order = jnp.argsort(eid_f)
    e_sorted = eid_f[order]
    counts = jnp.bincount(eid_f, length=N_EXPERTS)
    starts = jnp.cumsum(counts) - counts
    padded = ((counts + EXPERT_BLOCK - 1) // EXPERT_BLOCK) * EXPERT_BLOCK
    pad_ends = jnp.cumsum(padded)
    pad_starts = pad_ends - padded
    dest = pad_starts[e_sorted] + (jnp.arange(A) - starts[e_sorted])
    P = A + N_EXPERTS * EXPERT_BLOCK
    NB = P // EXPERT_BLOCK
    buf_tok = jnp.full((P,), T, dtype=jnp.int32).at[dest].set(tok_f[order])
    buf_gate = jnp.zeros((P,), jnp.float32).at[dest].set(gate_f[order])
    blk_e = jnp.minimum(jnp.searchsorted(pad_ends, jnp.arange(NB) * EXPERT_BLOCK, side='right'), N_EXPERTS - 1)
    x_pad = jnp.concatenate([xt, jnp.zeros((1, D), xt.dtype)], axis=0)
    xb = x_pad[buf_tok].reshape(NB, EXPERT_BLOCK, D)

    def expert_block(args):
        xi, e = args
        return (jax.nn.silu(xi @ w_g[e]) * (xi @ w_u[e])) @ w_d[e]

    yb = lax.map(expert_block, (xb, blk_e)).reshape(P, D)
    y = jnp.zeros((T + 1, D), jnp.float32).at[buf_tok].add(yb.astype(jnp.float32) * buf_gate[:, None])[:T]
    return y.astype(h.dtype).reshape(B, S, D)


def setup_inputs(seed: int = 0) -> dict:
    key = jax.random.key(seed)
    ks = jax.random.split(key, 24)
    f32 = jnp.float32
    nrm = lambda k, shape, s: jax.random.normal(k, shape, f32) * s
    L = DEPTH
    return {
        'x': jax.random.normal(ks[0], (BATCH, SEQ, D_MODEL), f32),
        'norm1_g': 1.0 + nrm(ks[1], (L, D_MODEL), 0.05),
        'w_in': nrm(ks[2], (L, D_MODEL, IN_COLS), D_MODEL ** -0.5),
        'b_gate': nrm(ks[3], (L, 2 * D_MODEL), 0.1),
        'b_forget': 2.0 + nrm(ks[4], (L, FOX_HEADS), 0.5),
        'sgu_ln_g': 1.0 + nrm(ks[5], (L, SGU_WIDTH), 0.05),
        'sgu_ln_b': nrm(ks[6], (L, SGU_WIDTH), 0.05),
        'w_spatial': nrm(ks[7], (L, SGU_GROUPS, CHUNK, CHUNK), CHUNK ** -0.5),
        'b_spatial': 1.0 + nrm(ks[8], (L, SGU_GROUPS, CHUNK), 0.1),
        'q_norm_g': 1.0 + nrm(ks[9], (L, HEAD_DIM), 0.05),
        'k_norm_g': 1.0 + nrm(ks[10], (L, HEAD_DIM), 0.05),
        'w_proj_sgu': nrm(ks[11], (L, SGU_WIDTH, D_MODEL), SGU_WIDTH ** -0.5),
        'w_proj_fox': nrm(ks[12], (L, FOX_WIDTH, D_MODEL), FOX_WIDTH ** -0.5),
        'w_out': nrm(ks[13], (L, D_MODEL, D_MODEL), D_MODEL ** -0.5),
        'norm2_g': 1.0 + nrm(ks[14], (L, D_MODEL), 0.05),
        'w_router_group': nrm(ks[15], (L, D_MODEL, N_GROUPS), D_MODEL ** -0.5),
        'b_router_group': nrm(ks[16], (L, N_GROUPS), 0.01),
        'w_router_expert': nrm(ks[17], (L, D_MODEL, N_EXPERTS), D_MODEL ** -0.5),
        'b_router_expert': nrm(ks[18], (L, N_EXPERTS), 0.01),
        'w_expert_gate': nrm(ks[19], (L, N_EXPERTS, D_MODEL, D_EXPERT), D_MODEL ** -0.5),
        'w_expert_up': nrm(ks[20], (L, N_EXPERTS, D_MODEL, D_EXPERT), D_MODEL ** -0.5),
        'w_expert_down': nrm(ks[21], (L, N_EXPERTS, D_EXPERT, D_MODEL), D_EXPERT ** -0.5),
    }


def reference(x, norm1_g, w_in, b_gate, b_forget, sgu_ln_g, sgu_ln_b, w_spatial, b_spatial,
              q_norm_g, k_norm_g, w_proj_sgu, w_proj_fox, w_out, norm2_g,
              w_router_group, b_router_group, w_router_expert, b_router_expert,
              w_expert_gate, w_expert_up, w_expert_down):
    B, S, D = x.shape
    for l in range(DEPTH):
        h = rms_norm(x, norm1_g[l])
        z = h @ w_in[l]
        u = jax.nn.gelu(z[..., OFF_U:OFF_V])
        v = jax.nn.gelu(z[..., OFF_V:OFF_Q])
        q = z[..., OFF_Q:OFF_K].reshape(B, S, FOX_HEADS, HEAD_DIM)
        k = z[..., OFF_K:OFF_VA].reshape(B, S, FOX_HEADS, HEAD_DIM)
        va = z[..., OFF_VA:OFF_F].reshape(B, S, FOX_HEADS, HEAD_DIM)
        f_logit = z[..., OFF_F:OFF_GATE] + b_forget[l]
        gates = jax.nn.sigmoid(z[..., OFF_GATE:] + b_gate[l])
        g_sgu = gates[..., :D]
        g_fox = gates[..., D:]
        y_sgu = chunked_sgu(u, v, sgu_ln_g[l], sgu_ln_b[l], w_spatial[l], b_spatial[l]) @ w_proj_sgu[l]
        y_fox = forgetting_attention(q, k, va, f_logit, q_norm_g[l], k_norm_g[l]) @ w_proj_fox[l]
        x = x + (g_sgu * y_sgu + g_fox * y_fox) @ w_out[l]
        h2 = rms_norm(x, norm2_g[l])
        x = x + hierarchical_moe(h2, w_router_group[l], b_router_group[l], w_router_expert[l],
                                 b_router_expert[l], w_expert_gate[l], w_expert_up[l], w_expert_down[l])
    return x
```

```python
import numpy as np
import concourse.bass as bass
import concourse.mybir as mybir
from concourse.alu_op_type import AluOpType as ALU
from concourse.bass_utils import run_bass_kernel_spmd

F32 = mybir.dt.float32
BF16 = mybir.dt.bfloat16
AF = mybir.ActivationFunctionType
AX = mybir.AxisListType
ENGS = ("pe", "act", "dve", "pool", "sp")
KB = 1024
EPS = 1e-6
SCALE = 128 ** -0.5
BIG = 1.0e4


class Buf:
    __slots__ = ("name", "w", "r")

    def __init__(self, name=""):
        self.name = name
        self.w = None
        self.r = []


def alias(new_bufs, old_bufs):
    evs = []
    for o in old_bufs:
        if o.w is not None:
            evs.append(o.w)
        evs.extend(o.r)
    red = {}
    for k, v in evs:
        if red.get(k, 0) < v:
            red[k] = v
    evs = list(red.items())
    for n in new_bufs:
        n.r = list(n.r) + evs


class Sched:
    def __init__(self, nc):
        self.nc = nc
        self.prog = {e: [] for e in ENGS}
        self.cnt = {}
        self.waited = {e: {} for e in ENGS}
        self.sems = {}
        self.same_engine_sync = {"act": True, "dve": True, "pool": True, "pe": False, "sp": False}

    def sem(self, key):
        if key not in self.sems:
            self.sems[key] = self.nc.alloc_semaphore(name="s%d" % len(self.sems))
            self.cnt[key] = 0
        return self.sems[key]

    def _deps(self, eng, reads, writes):
        deps = {}

        def add(ev):
            if ev is None:
                return
            k, v = ev
            if deps.get(k, 0) < v:
                deps[k] = v

        for b in reads:
            add(b.w)
        for b in writes:
            add(b.w)
            for r in b.r:
                add(r)
        waits = []
        for k, v in deps.items():
            if k == eng and not self.same_engine_sync.get(eng, True):
                continue
            if self.waited[eng].get(k, 0) >= v:
                continue
            self.waited[eng][k] = v
            waits.append((k, v))
        return waits

    def op(self, eng, fn, reads=(), writes=()):
        self.sem(eng)
        waits = self._deps(eng, reads, writes)
        self.cnt[eng] += 1
        ev = (eng, self.cnt[eng])
        self.prog[eng].append((waits, fn, (eng, 1)))
        for b in reads:
            b.r.append(ev)
        for b in writes:
            b.w = ev
            b.r = []
        return ev

    def dma(self, queue, fn, reads=(), writes=(), semkey=None, chain=False):
        self.sem(semkey)
        waits = [] if chain else self._deps(queue, reads, writes)
        self.cnt[semkey] += 16
        ev = (semkey, self.cnt[semkey])
        self.prog[queue].append((waits, fn, (semkey, 16)))
        for b in reads:
            b.r.append(ev)
        for b in writes:
            b.w = ev
            b.r = []
        return ev

    def final_wait(self, queue, bufs):
        waits = self._deps(queue, bufs, ())
        self.prog[queue].append((waits, None, None))

    def emit(self, eng, h):
        for waits, fn, inc in self.prog[eng]:
            for k, v in waits:
                h.wait_ge(self.sems[k], v)
            if fn is None:
                continue
            ins = fn(h)
            ins.then_inc(self.sems[inc[0]], inc[1])

    def run(self):
        nc = self.nc
        with nc.Block() as block:
            @block.tensor
            def _(e):
                self.emit("pe", e)

            @block.scalar
            def _(e):
                self.emit("act", e)

            @block.vector
            def _(e):
                self.emit("dve", e)

            @block.gpsimd
            def _(e):
                self.emit("pool", e)

            @block.sync
            def _(e):
                self.emit("sp", e)


OFF_U, OFF_V, OFF_Q, OFF_K, OFF_VA, OFF_F, OFF_GATE = 0, 1024, 2048, 3072, 4096, 5120, 5128
IN_COLS = 9224


def build(dbg=(), upto=99):
    nc = bass.Bass("TRN2", target_bir_lowering=False)

    def din(name, shape):
        return nc.dram_tensor(name, list(shape), F32, kind="ExternalInput").ap()

    xo = din("xo", [1024, 2048])
    xp = din("xp", [1024, 2048])
    maskb_d = din("maskb", [128, 1])
    g1B_d = din("g1B", [128, 2048])
    g2B_d = din("g2B", [128, 2048])
    w_in = din("w_in", [2048, IN_COLS])
    bgT_d = din("bgT", [128, 32])
    bfB_d = din("bfB", [128, 128])
    lngB_d = din("lngB", [128, 1024])
    lnbB_d = din("lnbB", [128, 1024])
    wsT_d = din("wsT", [128, 1024])
    bspB_d = din("bspB", [128, 1024])
    qg_d = din("qg", [128, 1])
    kg_d = din("kg", [128, 1])
    wps_d = din("wps", [1024, 2048])
    wpf_d = din("wpf", [1024, 2048])
    wout_d = din("wout", [2048, 2048])
    wr_d = din("wr", [2048, 36])
    brB_d = din("brB", [128, 36])
    weg_d = din("weg", [32, 2048, 512])
    weu_d = din("weu", [32, 2048, 512])
    wed_d = din("wed", [32, 512, 2048])
    out_d = nc.dram_tensor("out", [1024, 2048], F32, kind="ExternalOutput").ap()
    dbg_d = {}
    for name, shape in dbg:
        dbg_d[name] = nc.dram_tensor("dbg_" + name, list(shape), F32, kind="ExternalOutput").ap()

    S = Sched(nc)
    arena_cm = nc.sbuf_tensor("arena", [128, 207 * KB // 4], F32)
    ps_cm = nc.psum_tensor("ps", [128, 8, 512], F32)
    with arena_cm as arena, ps_cm as ps:
        def cv(off, nbytes, dt=F32):
            a = arena[:, off // 4:(off + nbytes) // 4]
            return a if dt == F32 else a.bitcast(dt)

        A0, A1, A2, A3, RING, CR, TR = 0, 32 * KB, 64 * KB, 96 * KB, 128 * KB, 176 * KB, 184 * KB
        bps = [Buf("ps%d" % i) for i in range(8)]

        def psf(b0, n=1):
            v = ps[:, b0:b0 + n, :]
            return v.rearrange("p a n -> p (a n)")

        c = CR
        ident_bf = cv(c, 256, BF16); c += 256
        negmask_bf = cv(c, 256, BF16); c += 256
        ones_bf = cv(c, 256, BF16); c += 256
        ident_f = cv(c, 512); c += 512
        triU_f = cv(c, 512); c += 512
        triS_f = cv(c, 512); c += 512
        ones_f = cv(c, 512); c += 512
        iota_f = cv(c, 512); c += 512
        bgT = cv(c, 128); c += 128
        bfB = cv(c, 512); c += 512
        smalls = cv(c, 32); c += 32
        pidx, qg, kg, maskb, epsc, zeroc, onec = [smalls[:, i:i + 1] for i in range(7)]
        wf = cv(c, 256, BF16).rearrange("p (k n) -> p k n", k=16); c += 256
        fz = cv(c, 512); c += 512
        spt = cv(c, 512); c += 512
        Cc = cv(c, 512); c += 512
        pre = cv(c, 512); c += 512
        Cm = cv(c, 512); c += 512
        offT = cv(TR + 12 * KB, 2 * KB, BF16)
        selAll = cv(TR + 14 * KB, 2 * KB, BF16).rearrange("p (h j) -> p h j", h=8)
        iotaH = cv(TR + 16 * KB, 4 * KB)
        ssv = cv(c, 64); c += 64
        rtv = cv(c, 64); c += 64
        rstdv = cv(c, 64); c += 64
        assert c <= CR + 8 * KB, c - CR
        b_const = Buf("const")
        b_iota = Buf("iota")

        def ld_const(dst, src, key):
            S.dma("sp", lambda e: e.dma_start(out=dst, in_=src), writes=[b_const], semkey="c_" + key)

        S.op("pool", lambda e: e.iota(iota_f, [[1, 128]], base=0, channel_multiplier=0, allow_small_or_imprecise_dtypes=True), writes=[b_iota])
        S.op("pool", lambda e: e.iota(pidx, [[0, 1]], base=0, channel_multiplier=1, allow_small_or_imprecise_dtypes=True), writes=[b_iota])
        ld_const(qg, qg_d, "qg")
        ld_const(kg, kg_d, "kg")
        ld_const(maskb, maskb_d, "maskb")
        ld_const(bgT, bgT_d, "bgT")
        ld_const(bfB, bfB_d, "bfB")
        S.op("dve", lambda e: e.memset(epsc, EPS), reads=[b_iota], writes=[b_const])
        S.op("dve", lambda e: e.memset(zeroc, 0.0), writes=[b_const])
        S.op("dve", lambda e: e.memset(onec, 1.0), writes=[b_const])
        S.op("dve", lambda e: e.memset(ones_f, 1.0), writes=[b_const])
        S.op("dve", lambda e: e.memset(ones_bf, 1.0), writes=[b_const])
        S.op("dve", lambda e: e.tensor_scalar(ident_f, iota_f, pidx, None, ALU.is_equal), reads=[b_iota], writes=[b_const])
        S.op("dve", lambda e: e.tensor_scalar(ident_bf, iota_f, pidx, None, ALU.is_equal), reads=[b_iota], writes=[b_const])
        S.op("dve", lambda e: e.tensor_scalar(triU_f, iota_f, pidx, None, ALU.is_ge), reads=[b_iota], writes=[b_const])
        S.op("dve", lambda e: e.tensor_scalar(negmask_bf, iota_f, pidx, -30000.0, ALU.is_lt, ALU.mult), reads=[b_iota], writes=[b_const])
        S.op("dve", lambda e: e.tensor_scalar(triS_f, iota_f, pidx, None, ALU.is_gt), reads=[b_iota], writes=[b_const])
        S.dma("pool", lambda e: e.dma_start(out=wf, in_=w_in[:, OFF_F:OFF_F + 8].rearrange("(k p) n -> p k n", p=128)),
              writes=[b_const], semkey="c_wf")

        NS8 = 6
        b8 = [Buf("r8_%d" % i) for i in range(NS8)]
        b16 = [Buf("r16_%d" % i) for i in range(NS8 // 2)]
        ap8 = [cv(RING + i * 8 * KB, 8 * KB, BF16) for i in range(NS8)]
        ap16 = [cv(RING + i * 16 * KB, 16 * KB, BF16) for i in range(NS8 // 2)]
        units = []

        def u_cols(src, c0, ncols, K):
            parts = []
            for k0 in range(0, K, 4):
                parts.append((lambda s, k0=k0, K=K, ncols=ncols: s[:, 0:K * ncols].rearrange("p (k n) -> p k n", k=K)[:, k0:k0 + 4, :],
                              src[k0 * 128:(k0 + 4) * 128, c0:c0 + ncols].rearrange("(k p) n -> p k n", p=128)))
            return (2, parts)

        for blk in range(2):
            units.append(u_cols(w_in, OFF_K + blk * 512, 512, 16))
        for blk in range(2):
            units.append(u_cols(w_in, OFF_VA + blk * 512, 512, 16))
        for blk in range(2):
            units.append(u_cols(w_in, OFF_Q + blk * 512, 512, 16))
        for blk in range(2):
            units.append(u_cols(w_in, OFF_U + blk * 512, 512, 16))
        for blk in range(2):
            units.append(u_cols(w_in, OFF_V + blk * 512, 512, 16))
        for n in range(4):
            units.append(u_cols(w_in, OFF_GATE + n * 512, 512, 16))
            units.append(u_cols(w_in, OFF_GATE + 2048 + n * 512, 512, 16))
            pp = []
            for half, srcw in enumerate((wps_d, wpf_d)):
                for k0 in (0, 4):
                    pp.append((lambda s, half=half, k0=k0: s.rearrange("p (k n) -> p k n", k=16)[:, half * 8 + k0:half * 8 + k0 + 4, :],
                               srcw[k0 * 128:(k0 + 4) * 128, n * 512:(n + 1) * 512].rearrange("(k p) n -> p k n", p=128)))
            units.append((2, pp))
        for n in range(4):
            units.append(u_cols(wout_d, n * 512, 512, 16))
        for ex in range(32):
            for wsrc in (weg_d, weu_d):
                for half in range(2):
                    pp = []
                    for k0 in (0, 4):
                        r0 = half * 1024 + k0 * 128
                        pp.append((lambda s, k0=k0: s.rearrange("p (k n) -> p k n", k=8)[:, k0:k0 + 4, :],
                                   wsrc[ex][r0:r0 + 512, :].rearrange("(k p) n -> p k n", p=128)))
                    units.append((1, pp))
            for half in range(2):
                pp = []
                for k0 in range(2):
                    r0 = half * 256 + k0 * 128
                    pp.append((lambda s, k0=k0: s.rearrange("p (k n) -> p k n", k=2)[:, k0:k0 + 1, :],
                               wed_d[ex][r0:r0 + 128, :].rearrange("(k p) n -> p k n", p=128)))
                units.append((1, pp))
        ws = {"iu": 0, "tu": 0, "ipos": 0, "tpos": 0, "upos": {}}

        def ws_issue():
            while ws["iu"] < len(units):
                size, parts = units[ws["iu"]]
                if ws["ipos"] + size - ws["tpos"] > NS8:
                    return
                p = ws["ipos"] % NS8
                if size == 2:
                    assert p % 2 == 0
                    dst_ap, wb, key = ap16[p // 2], [b16[p // 2], b8[p], b8[p + 1]], ("ring16", p // 2)
                else:
                    alias([b8[p]], [b16[p // 2]])
                    dst_ap, wb, key = ap8[p], [b8[p]], ("ring8", p)
                for pi, (dst_fn, src) in enumerate(parts):
                    dst = dst_fn(dst_ap)
                    S.dma("pool", (lambda e, dst=dst, src=src: e.dma_start(out=dst, in_=src)), writes=wb, semkey=key, chain=(pi > 0))
                ws["upos"][ws["iu"]] = p
                ws["iu"] += 1
                ws["ipos"] += size

        def ws_take():
            u = ws["tu"]
            assert u < ws["iu"], "weight unit not issued yet"
            size, _ = units[u]
            p = ws["upos"][u]
            ws["tu"] += 1
            ws["pending_release"] = ws.get("pending_release", [])
            ws["pending_release"].append(size)
            if size == 2:
                return ap16[p // 2], b16[p // 2]
            return ap8[p], b8[p]

        def ws_release():
            size = ws["pending_release"].pop(0)
            ws["tpos"] += size
            ws_issue()

        ws_issue()

        hTp = cv(A0, 32 * KB, BF16).rearrange("p (k t) -> p k t", k=16)
        hTo = cv(A1, 32 * KB, BF16).rearrange("p (k t) -> p k t", k=16)

        def hTs(k, t0, n):
            return hTp[:, k, t0:t0 + n] if t0 < 1024 else hTo[:, k, t0 - 1024:t0 - 1024 + n]

        def hTall(i):
            return hTp[:, :, i * 128:(i + 1) * 128] if i < 8 else hTo[:, :, (i - 8) * 128:(i - 7) * 128]
        b_hT = [Buf("hT%d" % i) for i in range(16)]
        KT = cv(A2, 32 * KB, BF16).rearrange("p (h t) -> p h t", h=8)
        b_KT2 = [[Buf("KT%d_%d" % (h, g)) for g in range(4)] for h in range(8)]
        b_KT = [b for row in b_KT2 for b in row]
        Vv = cv(A3, 32 * KB, BF16).rearrange("p (i n) -> p i n", i=16)
        b_V2 = [[Buf("V%d_%d" % (i, g)) for g in range(2)] for i in range(16)]
        b_V = [b for row in b_V2 for b in row]

        g1B = cv(A2, 8 * KB)
        xs = [cv(A3 + i * 8 * KB, 8 * KB) for i in range(3)]
        xn = [cv(A3 + 24 * KB + i * 4 * KB, 4 * KB, BF16) for i in range(2)]
        junk = cv(TR + 16 * KB, 4 * KB, BF16)
        b_g1B, b_junk = Buf("g1B"), Buf("junk")
        b_xs = [Buf(), Buf(), Buf()]
        b_xn = [Buf(), Buf()]
        b_ssl = [Buf("ss%d" % i) for i in range(16)]
        S.dma("sp", lambda e: e.dma_start(out=g1B, in_=g1B_d), writes=[b_g1B], semkey="c_g1B")

        def rms_stats(src_ap, b_src, col, width):
            S.op("act", lambda e: e.activation(out=junk[:, 0:width], in_=src_ap, func=AF.Square, accum_out=ssv[:, col:col + 1]),
                 reads=[b_src], writes=[b_junk, b_ssl[col]])
            S.op("act", lambda e: e.activation(out=rtv[:, col:col + 1], in_=ssv[:, col:col + 1], func=AF.Sqrt, scale=1.0 / width, bias=epsc),
                 reads=[b_ssl[col], b_const], writes=[b_ssl[col]])
            S.op("dve", lambda e: e.reciprocal(rstdv[:, col:col + 1], rtv[:, col:col + 1]), reads=[b_ssl[col]], writes=[b_ssl[col]])

        def phase1_tile(i):
            sl = i % 2
            s3 = i % 3
            src = (xp if i < 8 else xo)[(i % 8) * 128:(i % 8 + 1) * 128, :]
            S.dma("sp", lambda e: e.dma_start(out=xs[s3], in_=src), writes=[b_xs[s3]], semkey=("xs", s3))
            rms_stats(xs[s3], b_xs[s3], i, 2048)
            S.op("dve", lambda e: e.scalar_tensor_tensor(out=xn[sl], in0=xs[s3], scalar=rstdv[:, i:i + 1], in1=g1B, op0=ALU.mult, op1=ALU.mult),
                 reads=[b_xs[s3], b_ssl[i], b_g1B], writes=[b_xn[sl]])
            pst = ps[:, 2 * sl:2 * sl + 2, :].bitcast(BF16).rearrange("p a (k n) -> p (a k) n", n=128)
            for k in range(16):
                S.op("pe", (lambda e, k=k: e.transpose(pst[:, k, :], xn[sl][:, k * 128:(k + 1) * 128], ident_bf)),
                     reads=[b_xn[sl], b_const], writes=[bps[2 * sl], bps[2 * sl + 1]])
            eng = "act" if i % 2 == 0 else "dve"
            if eng == "act":
                S.op("act", lambda e: e.activation(out=hTall(i), in_=pst, func=AF.Copy),
                     reads=[bps[2 * sl], bps[2 * sl + 1]], writes=[b_hT[i]])
            else:
                S.op("dve", lambda e: e.tensor_copy(hTall(i), pst),
                     reads=[bps[2 * sl], bps[2 * sl + 1]], writes=[b_hT[i]])

        for i in range(16):
            phase1_tile(i)

        dumps = []

        def dump(name, ap, bufs):
            if name in dbg_d:
                dumps.append((name, ap, bufs))

        alias(b_KT, [b_g1B])
        alias(b_V, b_xs + b_xn)
        tsq = [cv(TR + i * KB, KB, BF16) for i in range(2)]
        trt = [cv(TR + 2 * KB + i * 2 * KB, 2 * KB) for i in range(2)]
        b_tsq = [Buf(), Buf()]
        b_trt = [Buf(), Buf()]
        rot = {"a": 0, "b": 0, "u": 0}

        def qk_unit(wslot, b_w, hh, dstT, b_dst, h, t0, hoff, gcol):
            ba = rot["a"] % 4
            rot["a"] += 1
            bb = 4 + rot["b"] % 2
            rot["b"] += 1
            u = rot["u"] % 2
            rot["u"] += 1
            w3 = wslot.rearrange("p (k n) -> p k n", k=16)
            tiles = [b_hT[(hoff + t0) // 128 + j] for j in range(4)]
            for k in range(16):
                S.op("pe", (lambda e, k=k: e.matmul(ps[:, ba, :], w3[:, k, hh * 128:(hh + 1) * 128], hTs(k, hoff + t0, 512), start=(k == 0), stop=(k == 15))),
                     reads=[b_w] + tiles, writes=[bps[ba]])
            def norm():
                S.op("act", lambda e: e.activation(out=tsq[u], in_=ps[:, ba, :], func=AF.Square), reads=[bps[ba]], writes=[b_tsq[u]])
                S.op("pe", lambda e: e.matmul(ps[:, bb, :], ones_bf, tsq[u], start=True, stop=True), reads=[b_tsq[u], b_const], writes=[bps[bb]])
                S.op("act", lambda e: e.activation(out=trt[u], in_=ps[:, bb, :], func=AF.Ln, scale=1.0 / 128, bias=epsc), reads=[bps[bb], b_const], writes=[b_trt[u]])
                S.op("act", lambda e: e.activation(out=trt[u], in_=trt[u], func=AF.Exp, scale=-0.5), reads=[b_trt[u]], writes=[b_trt[u]])
                S.op("dve", lambda e: e.scalar_tensor_tensor(out=dstT[:, h, t0:t0 + 512], in0=ps[:, ba, :], scalar=gcol, in1=trt[u], op0=ALU.mult, op1=ALU.mult),
                     reads=[bps[ba], b_trt[u], b_const], writes=[b_dst])
            prev = qkp["pending"]
            qkp["pending"] = norm
            if prev is not None:
                prev()

        qkp = {"pending": None}

        def qk_flush():
            if qkp["pending"] is not None:
                qkp["pending"]()
                qkp["pending"] = None

        for blk in range(2):
            wslot, b_w = ws_take()
            for hh in range(4):
                for tg in range(4):
                    qk_unit(wslot, b_w, hh, KT, b_KT2[blk * 4 + hh][tg], blk * 4 + hh, tg * 512, 0, kg)
            ws_release()
        qk_flush()
        evi = [0]

        def evac(out_ap, in_ap, reads, writes, eng=None):
            evi[0] += 1
            if eng == "act" or (eng is None and evi[0] % 2 == 0):
                S.op("act", lambda e: e.activation(out=out_ap, in_=in_ap, func=AF.Copy), reads=reads, writes=writes)
            else:
                S.op("dve", lambda e: e.tensor_copy(out_ap, in_ap), reads=reads, writes=writes)

        def v_unit(wslot, b_w, blk, i):
            w3 = wslot.rearrange("p (k n) -> p k n", k=16)
            ba = rot["a"] % 4
            rot["a"] += 1
            for k in range(16):
                S.op("pe", (lambda e, k=k: e.matmul(ps[:, ba, :], hTs(k, i * 128, 128), w3[:, k, :], start=(k == 0), stop=(k == 15))),
                     reads=[b_w, b_hT[i]], writes=[bps[ba]])
            evac(Vv[:, i, blk * 512:(blk + 1) * 512], ps[:, ba, :], [bps[ba]], [b_V2[i][blk]])

        for blk in range(2):
            wslot, b_w = ws_take()
            for i in range(16):
                v_unit(wslot, b_w, blk, i)
            ws_release()
        b_f = Buf("f")
        for i in range(16):
            for k in range(16):
                S.op("pe", (lambda e, k=k, i=i: e.matmul(ps[:, 7, i * 8:(i + 1) * 8], hTs(k, i * 128, 128), wf[:, k, :], start=(k == 0), stop=(k == 15))),
                     reads=[b_const, b_hT[i]], writes=[bps[7]])
        S.op("dve", lambda e: e.tensor_tensor(out=fz, in0=ps[:, 7, 0:128], in1=bfB, op=ALU.add), reads=[bps[7], b_const], writes=[b_f])
        S.op("act", lambda e: e.activation(out=fz, in_=fz, func=AF.Exp, scale=-1.0), reads=[b_f], writes=[b_f])
        S.op("act", lambda e: e.activation(out=spt, in_=fz, func=AF.Ln, bias=onec), reads=[b_f, b_const], writes=[b_f])
        b_C = Buf("C")
        for i in range(16):
            S.op("pe", (lambda e, i=i: e.matmul(ps[:, 5, i * 8:(i + 1) * 8], ones_f, spt[:, i * 8:(i + 1) * 8], start=True, stop=True)),
                 reads=[b_f, b_const], writes=[bps[5]])
        for i in range(16):
            S.op("pe", (lambda e, i=i: e.matmul(ps[:, 6, i * 8:(i + 1) * 8], triU_f, spt[:, i * 8:(i + 1) * 8], start=True, stop=True)),
                 reads=[b_f, b_const], writes=[bps[6]])
        S.op("dve", lambda e: e.tensor_copy(fz, ps[:, 5, 0:128]), reads=[bps[5], b_f], writes=[b_f])
        S.op("dve", lambda e: e.memset(pre[:, 0:8], 0.0), writes=[b_C])
        for i in range(1, 16):
            S.op("dve", (lambda e, i=i: e.tensor_tensor(out=pre[:, i * 8:(i + 1) * 8], in0=pre[:, (i - 1) * 8:i * 8], in1=fz[:, (i - 1) * 8:i * 8], op=ALU.add)),
                 reads=[b_f, b_C], writes=[b_C])
        S.op("dve", lambda e: e.tensor_tensor(out=Cc, in0=ps[:, 6, 0:128], in1=pre, op=ALU.add), reads=[bps[6], b_C], writes=[b_C])
        b_bt = Buf("attbias")
        S.op("dve", lambda e: e.tensor_scalar(Cm[:, 0:64], Cc[:, 0:64], maskb, None, ALU.add), reads=[b_C, b_const], writes=[b_bt])
        S.op("dve", lambda e: e.tensor_copy(Cm[:, 64:128], Cc[:, 64:128]), reads=[b_C], writes=[b_bt])
        b_off = Buf("offT")
        alias([b_off], b_tsq + b_trt + [b_junk])
        S.op("pool", lambda e: e.iota(iotaH, [[1, 8], [0, 128]], base=0, channel_multiplier=0, allow_small_or_imprecise_dtypes=True), writes=[b_off])
        S.op("dve", lambda e: e.tensor_scalar(selAll.rearrange("p h j -> p (h j)"), iotaH, pidx, None, ALU.is_equal), reads=[b_off, b_iota], writes=[b_off])
        S.op("dve", lambda e: e.memset(offT, 0.0), writes=[b_off])
        psO = psf(4, 2)
        for i in range(8):
            S.op("pe", (lambda e, i=i: e.transpose(psO[0:8, i * 128:(i + 1) * 128], Cc[:, (8 + i) * 8:(9 + i) * 8], ident_f)),
                 reads=[b_C, b_const], writes=[bps[4 + i // 4]])
        S.op("dve", lambda e: e.tensor_scalar(offT[0:8, :], psO[0:8, :], -1.0 / SCALE, None, ALU.mult), reads=[bps[4], bps[5], b_off], writes=[b_off])
        dump("KT", KT, b_KT)
        dump("V", Vv, b_V)
        dump("Cc", Cc, [b_C])

        QT = cv(A0, 16 * KB, BF16).rearrange("p (h t) -> p h t", h=8)
        b_QT2 = [[Buf("QT%d_%d" % (h, g)) for g in range(2)] for h in range(8)]
        b_QT = [b for row in b_QT2 for b in row]
        alias(b_QT, b_hT[0:8])
        for blk in range(2):
            wslot, b_w = ws_take()
            for hh in range(4):
                for tg in range(2):
                    qk_unit(wslot, b_w, hh, QT, b_QT2[blk * 4 + hh][tg], blk * 4 + hh, tg * 512, 1024, qg)
            ws_release()
        qk_flush()
        dump("QT", QT, b_QT)
        if upto <= 4:
            return finish(nc, S, dumps, dbg_d)


        oT = cv(A0 + 16 * KB, 16 * KB, BF16).rearrange("p (h t) -> p h t", h=8)
        b_oT = [Buf("oT%d" % h) for h in range(8)]
        alias(b_oT, b_hT[0:8])
        PT = [cv(TR + i * 2 * KB, 2 * KB, BF16) for i in range(3)]
        b_PT = [Buf() for _ in range(3)]
        rinv2 = [cv(TR + 6 * KB, 2 * KB), cv(TR + 20 * KB, 2 * KB)]
        b_rinv2 = [Buf(), Buf()]
        b_rinv = b_rinv2[0]
        alias(b_PT + b_rinv2, b_tsq + b_trt)

        PTq = [cv(TR + i * KB, KB, BF16) for i in range(6)]
        b_PT.extend([Buf(), Buf(), Buf()])
        b_PTq = b_PT
        alias(b_PTq[3:], b_tsq + b_trt)

        def step_geom(qh, kt):
            q0 = max(qh * 512, max(0, kt - 8) * 128)
            q1 = (qh + 1) * 512
            return q0, q1, q0 - qh * 512, q1 - qh * 512

        def att_S(h, qh, kt, sbank):
            q0, q1, l0, l1 = step_geom(qh, kt)
            dq = (kt - 8) * 128
            diag = (kt >= 8 and qh * 512 <= dq < (qh + 1) * 512)
            wr = [bps[sbank]]
            S.op("pe", lambda e: e.matmul(ps[:, sbank, l0:l1], KT[:, h, kt * 128:(kt + 1) * 128], QT[:, h, q0:q1], start=True, stop=False),
                 reads=[b_KT2[h][kt // 4], b_QT2[h][qh]], writes=wr)
            S.op("pe", lambda e: e.matmul(ps[:, sbank, l0:l1], selAll[:, h, :], offT[:, q0:q1], start=False, stop=(not diag)), reads=[b_off], writes=wr)
            if diag:
                S.op("pe", lambda e: e.matmul(ps[:, sbank, dq - qh * 512:dq - qh * 512 + 128], ident_bf, negmask_bf, start=False, stop=True), reads=[b_const], writes=wr)

        def att_P(h, qh, kt, sbank, slot):
            q0, q1, l0, l1 = step_geom(qh, kt)
            S.op("act", lambda e: e.activation(out=PTq[slot][:, l0:l1], in_=ps[:, sbank, l0:l1], func=AF.Exp, scale=SCALE, bias=Cm[:, kt * 8 + h:kt * 8 + h + 1]),
                 reads=[bps[sbank], b_bt], writes=[b_PTq[slot]])

        def att_V(h, qh, kt, slot):
            q0, q1, l0, l1 = step_geom(qh, kt)
            first = (kt == 0)
            last = (kt == (11 if qh == 0 else 15))
            S.op("pe", lambda e: e.matmul(ps[:, 4 + qh, l0:l1], Vv[:, kt, h * 128:(h + 1) * 128], PTq[slot][:, l0:l1], start=first, stop=last),
                 reads=[b_V2[kt][h // 4], b_PTq[slot]], writes=[bps[4 + qh]])
            S.op("pe", lambda e: e.matmul(ps[:, 6 + qh, l0:l1], ones_bf, PTq[slot][:, l0:l1], start=first, stop=last),
                 reads=[b_PTq[slot], b_const], writes=[bps[6 + qh]])

        def att_fin(h, qh):
            S.op("dve", lambda e: e.reciprocal(rinv2[qh], ps[:, 6 + qh, :]), reads=[bps[6 + qh]], writes=[b_rinv2[qh]])
            S.op("dve", lambda e: e.tensor_tensor(out=oT[:, h, qh * 512:(qh + 1) * 512], in0=ps[:, 4 + qh, :], in1=rinv2[qh], op=ALU.mult),
                 reads=[bps[4 + qh], b_rinv2[qh]], writes=[b_oT[h]])

        steps = [(h, qh, kt) for h in range(8) for qh in range(2) for kt in range(12 if qh == 0 else 16)]
        LOOK = 3
        for idx in range(min(LOOK, len(steps))):
            att_S(*steps[idx], idx % 4)
        for idx, (h, qh, kt) in enumerate(steps):
            if idx + LOOK < len(steps):
                att_S(*steps[idx + LOOK], (idx + LOOK) % 4)
            att_P(h, qh, kt, idx % 4, idx % 6)
            att_V(h, qh, kt, idx % 6)
            if kt == (11 if qh == 0 else 15):
                att_fin(h, qh)
        dump("oT", oT, b_oT)
        if upto <= 5:
            return finish(nc, S, dumps, dbg_d)

        uT = cv(A2, 16 * KB, BF16).rearrange("p (g t) -> p g t", g=8)
        vn = cv(A2 + 16 * KB, 16 * KB, BF16).rearrange("p (i n) -> p i n", i=8)
        b_uT = [Buf("uT%d" % g) for g in range(8)]
        b_vn = [Buf("vn%d" % i) for i in range(8)]
        alias(b_uT + b_vn, b_KT)
        lngB = cv(A3, 4 * KB)
        lnbB = cv(A3 + 4 * KB, 4 * KB)
        bspB = cv(A3 + 8 * KB, 4 * KB)
        wsTf = cv(A3 + 12 * KB, 4 * KB)
        wsTm = cv(A3 + 16 * KB, 2 * KB, BF16).rearrange("p (g t) -> p g t", g=8)
        vg = [cv(A3 + 18 * KB + i * 4 * KB, 4 * KB) for i in range(2)]
        tmpS = cv(A3 + 26 * KB, 4 * KB)
        b_sguc, b_wsm, b_tmpS = Buf("sguc"), Buf("wsm"), Buf("tmpS")
        b_vg = [Buf(), Buf()]
        alias([b_sguc, b_wsm, b_tmpS] + b_vg, b_V)
        for dst, src, key in ((lngB, lngB_d, "lng"), (lnbB, lnbB_d, "lnb"), (bspB, bspB_d, "bsp"), (wsTf, wsT_d, "wst")):
            S.dma("sp", (lambda e, dst=dst, src=src: e.dma_start(out=dst, in_=src)), writes=[b_sguc], semkey="c_sgu", chain=(key != "lng"))
        for g in range(8):
            S.op("dve", (lambda e, g=g: e.tensor_tensor(out=wsTm[:, g, :], in0=wsTf[:, g * 128:(g + 1) * 128], in1=triU_f, op=ALU.mult)),
                 reads=[b_sguc, b_const], writes=[b_wsm])
        bst = cv(TR + 8 * KB, 96)
        mv = cv(TR + 8 * KB + 96, 32)
        b_st = Buf("st")

        def u_unit(wslot, b_w, gg, g, tg):
            w3 = wslot.rearrange("p (k n) -> p k n", k=16)
            ba = rot["a"] % 4
            rot["a"] += 1
            for k in range(16):
                S.op("pe", (lambda e, k=k: e.matmul(ps[:, ba, :], w3[:, k, gg * 128:(gg + 1) * 128], hTo[:, k, tg * 512:(tg + 1) * 512], start=(k == 0), stop=(k == 15))),
                     reads=[b_w] + b_hT[8 + tg * 4:12 + tg * 4], writes=[bps[ba]])
            S.op("act", lambda e: e.activation(out=uT[:, g, tg * 512:(tg + 1) * 512], in_=ps[:, ba, :], func=AF.Gelu_apprx_tanh), reads=[bps[ba]], writes=[b_uT[g]])

        for blk in range(2):
            wslot, b_w = ws_take()
            for gg in range(4):
                for tg in range(2):
                    u_unit(wslot, b_w, gg, blk * 4 + gg, tg)
            ws_release()

        def v_proj_ln(w0, b_w0, w1, b_w1, i):
            sl = i % 2
            for blk, (wslot, b_w) in enumerate(((w0, b_w0), (w1, b_w1))):
                w3 = wslot.rearrange("p (k n) -> p k n", k=16)
                ba = 4 + 2 * sl + blk
                for k in range(16):
                    S.op("pe", (lambda e, k=k, w3=w3, ba=ba: e.matmul(ps[:, ba, :], hTo[:, k, i * 128:(i + 1) * 128], w3[:, k, :], start=(k == 0), stop=(k == 15))),
                         reads=[b_w, b_hT[8 + i]], writes=[bps[ba]])
            S.op("act", lambda e: e.activation(out=vg[sl], in_=psf(4 + 2 * sl, 2), func=AF.Gelu_apprx_tanh), reads=[bps[4 + 2 * sl], bps[5 + 2 * sl]], writes=[b_vg[sl]])
            bs_, mv_ = bst[:, sl * 12:sl * 12 + 12], mv[:, sl * 4:sl * 4 + 4]
            S.op("dve", lambda e: e.bn_stats(bs_[:, 0:6], vg[sl][:, 0:512]), reads=[b_vg[sl]], writes=[b_st2[sl]])
            S.op("dve", lambda e: e.bn_stats(bs_[:, 6:12], vg[sl][:, 512:1024]), reads=[b_vg[sl]], writes=[b_st2[sl]])
            S.op("dve", lambda e: e.bn_aggr(mv_[:, 0:2], bs_.rearrange("p (a b) -> p a b", b=6)), reads=[b_st2[sl]], writes=[b_st2[sl]])
            S.op("act", lambda e: e.activation(out=mv_[:, 2:3], in_=mv_[:, 1:2], func=AF.Sqrt, scale=1.0, bias=epsc), reads=[b_st2[sl], b_const], writes=[b_st2[sl]])
            S.op("dve", lambda e: e.reciprocal(mv_[:, 3:4], mv_[:, 2:3]), reads=[b_st2[sl]], writes=[b_st2[sl]])
            S.op("dve", lambda e: e.tensor_scalar(vg[sl], vg[sl], mv_[:, 0:1], mv_[:, 3:4], ALU.subtract, ALU.mult), reads=[b_st2[sl], b_vg[sl]], writes=[b_vg[sl]])
            S.op("dve", lambda e: e.tensor_tensor(out=vg[sl], in0=vg[sl], in1=lngB, op=ALU.mult), reads=[b_vg[sl], b_sguc], writes=[b_vg[sl]])
            S.op("dve", lambda e: e.tensor_tensor(out=vn[:, i, :], in0=vg[sl], in1=lnbB, op=ALU.add), reads=[b_vg[sl], b_sguc], writes=[b_vn[i]])

        def v_spatial(i):
            sl = i % 2
            psX = psf(0 + 2 * sl, 2)
            for g in range(8):
                S.op("pe", (lambda e, g=g: e.matmul(psX[:, g * 128:(g + 1) * 128], vn[:, i, g * 128:(g + 1) * 128], wsTm[:, g, :], start=True, stop=True)),
                     reads=[b_vn[i], b_wsm], writes=[bps[2 * sl + g // 4]])
            S.op("dve", lambda e: e.tensor_tensor(out=tmpS, in0=psX, in1=bspB, op=ALU.add), reads=[bps[2 * sl], bps[2 * sl + 1], b_sguc], writes=[b_tmpS])
            S.op("dve", lambda e: e.tensor_tensor(out=uT[:, :, i * 128:(i + 1) * 128], in0=tmpS.rearrange("p (g t) -> p g t", g=8),
                                                  in1=uT[:, :, i * 128:(i + 1) * 128], op=ALU.mult),
                 reads=[b_tmpS] + b_uT, writes=b_uT)

        b_st2 = [b_st, Buf("st2")]
        w0, b_w0 = ws_take()
        w1, b_w1 = ws_take()
        v_proj_ln(w0, b_w0, w1, b_w1, 0)
        for i in range(8):
            if i + 1 < 8:
                v_proj_ln(w0, b_w0, w1, b_w1, i + 1)
            v_spatial(i)
        ws_release()
        ws_release()
        dump("suT", uT, b_uT)
        if upto <= 6:
            return finish(nc, S, dumps, dbg_d)

        mT = cv(A3, 32 * KB, BF16).rearrange("p (c t) -> p c t", c=16)
        b_mT2 = [[Buf("mT%d_%d" % (c_, g)) for g in range(2)] for c_ in range(16)]
        b_mT = [b for row in b_mT2 for b in row]
        alias(b_mT, [b_sguc, b_wsm, b_tmpS] + b_vg)
        sgS = [cv(TR, 8 * KB, BF16).rearrange("p (c t) -> p c t", c=4), cv(A2 + 16 * KB, 8 * KB, BF16).rearrange("p (c t) -> p c t", c=4)]
        sgF = [cv(TR + 8 * KB, 8 * KB, BF16).rearrange("p (c t) -> p c t", c=4), cv(A2 + 24 * KB, 8 * KB, BF16).rearrange("p (c t) -> p c t", c=4)]
        m1t = [cv(A0 + i * 4 * KB, 2 * KB) for i in range(2)]
        m2t = [cv(A0 + i * 4 * KB + 2 * KB, 2 * KB) for i in range(2)]
        b_sgS3 = [[[Buf() for g in range(2)] for cc in range(4)] for p_ in range(2)]
        b_sgF3 = [[[Buf() for g in range(2)] for cc in range(4)] for p_ in range(2)]
        b_sgS = [[b for r in b_sgS3[p_] for b in r] for p_ in range(2)]
        b_sgF = [[b for r in b_sgF3[p_] for b in r] for p_ in range(2)]
        b_g7 = [Buf(), Buf()]
        alias(b_sgS[0] + b_sgS[1] + b_sgF[0] + b_sgF[1] + b_g7, b_PT + b_rinv2 + [b_st, b_st2[1], b_bt, b_off] + b_vn + b_QT)
        g7 = {"u": 0}

        def sig_unit(wg, b_wg, dst, b_dst, cc, bcol, tg):
            ba = rot["a"] % 8
            rot["a"] += 1
            wg3 = wg.rearrange("p (k n) -> p k n", k=16)
            tsl = slice(tg * 512, (tg + 1) * 512)
            csl = slice(cc * 128, (cc + 1) * 128)
            for k in range(16):
                S.op("pe", (lambda e, k=k: e.matmul(ps[:, ba, :], wg3[:, k, csl], hTo[:, k, tsl], start=(k == 0), stop=(k == 15))),
                     reads=[b_wg] + b_hT[8 + tg * 4:12 + tg * 4], writes=[bps[ba]])
            S.op("act", lambda e: e.activation(out=dst[:, cc, tsl], in_=ps[:, ba, :], func=AF.Sigmoid, bias=bgT[:, bcol:bcol + 1]), reads=[bps[ba], b_const], writes=[b_dst])

        def proj_unit(wpp, b_wpp, sS, b_sS, sF, b_sF, cc, c_, tg):
            u = g7["u"] % 2
            g7["u"] += 1
            b0 = rot["a"] % 8
            rot["a"] += 1
            b1 = rot["a"] % 8
            rot["a"] += 1
            wpp3 = wpp.rearrange("p (k n) -> p k n", k=16)
            tsl = slice(tg * 512, (tg + 1) * 512)
            csl = slice(cc * 128, (cc + 1) * 128)
            for k in range(8):
                S.op("pe", (lambda e, k=k: e.matmul(ps[:, b0, :], wpp3[:, k, csl], uT[:, k, tsl], start=(k == 0), stop=(k == 7))),
                     reads=[b_wpp, b_uT[k]], writes=[bps[b0]])
            for k in range(8):
                S.op("pe", (lambda e, k=k: e.matmul(ps[:, b1, :], wpp3[:, 8 + k, csl], oT[:, k, tsl], start=(k == 0), stop=(k == 7))),
                     reads=[b_wpp, b_oT[k]], writes=[bps[b1]])
            S.op("dve", lambda e: e.tensor_tensor(out=m1t[u], in0=sS[:, cc, tsl], in1=ps[:, b0, :], op=ALU.mult), reads=[b_sS, bps[b0]], writes=[b_g7[u]])
            S.op("dve", lambda e: e.tensor_tensor(out=m2t[u], in0=sF[:, cc, tsl], in1=ps[:, b1, :], op=ALU.mult), reads=[b_sF, bps[b1]], writes=[b_g7[u]])
            S.op("dve", lambda e: e.tensor_tensor(out=mT[:, c_, tsl], in0=m1t[u], in1=m2t[u], op=ALU.add), reads=[b_g7[u]], writes=[b_mT2[c_][tg]])

        for n in range(4):
            p = n % 2
            wgs, b_wgs = ws_take()
            for cc in range(4):
                for tg in range(2):
                    sig_unit(wgs, b_wgs, sgS[p], b_sgS3[p][cc][tg], cc, n * 4 + cc, tg)
            ws_release()
            wgf, b_wgf = ws_take()
            for cc in range(4):
                for tg in range(2):
                    sig_unit(wgf, b_wgf, sgF[p], b_sgF3[p][cc][tg], cc, 16 + n * 4 + cc, tg)
            ws_release()
            wpp, b_wpp = ws_take()
            for cc in range(4):
                for tg in range(2):
                    proj_unit(wpp, b_wpp, sgS[p], b_sgS3[p][cc][tg], sgF[p], b_sgF3[p][cc][tg], cc, n * 4 + cc, tg)
            ws_release()
        dump("mT", mT, b_mT)
        if upto <= 7:
            return finish(nc, S, dumps, dbg_d)

        x1 = cv(A0, 64 * KB).rearrange("p (i n) -> p i n", i=8)
        b_x1 = [Buf("x1_%d" % i) for i in range(8)]
        alias(b_x1, b_QT + b_oT + b_hT[8:16] + b_g7)
        for i in range(8):
            S.dma("sp", (lambda e, i=i: e.dma_start(out=x1[:, i, :], in_=xo[i * 128:(i + 1) * 128, :])), writes=[b_x1[i]], semkey=("x1", i))

        def out_unit(wo, b_wo, n, i):
            wo3 = wo.rearrange("p (k n) -> p k n", k=16)
            ba = rot["a"] % 8
            rot["a"] += 1
            for c_ in range(16):
                S.op("pe", (lambda e, c_=c_: e.matmul(ps[:, ba, :], mT[:, c_, i * 128:(i + 1) * 128], wo3[:, c_, :], start=(c_ == 0), stop=(c_ == 15))),
                     reads=[b_wo, b_mT2[c_][i // 4]], writes=[bps[ba]])
            S.op("dve", lambda e: e.tensor_tensor(out=x1[:, i, n * 512:(n + 1) * 512], in0=x1[:, i, n * 512:(n + 1) * 512], in1=ps[:, ba, :], op=ALU.add),
                 reads=[bps[ba], b_x1[i]], writes=[b_x1[i]])

        for n in range(4):
            wo, b_wo = ws_take()
            for i in range(8):
                out_unit(wo, b_wo, n, i)
            ws_release()
        dump("x1", x1, b_x1)
        if upto <= 8:
            return finish(nc, S, dumps, dbg_d)

        h2 = cv(A2, 32 * KB, BF16).rearrange("p (i n) -> p i n", i=8)
        b_h2 = [Buf("h2_%d" % i) for i in range(8)]
        alias(b_h2, b_uT + b_vn + b_sgS[1] + b_sgF[1])
        g2B = cv(A3, 8 * KB)
        wr3 = cv(A3 + 8 * KB, 2304).rearrange("p (k n) -> p k n", k=16)
        brB = cv(A3 + 8 * KB + 2304, 144)
        h2f_l = [cv(A3 + 12 * KB, 8 * KB), cv(TR + 6400, 8 * KB)]
        h2Tf_l = [cv(A3 + 20 * KB, 8 * KB), cv(TR + 6400 + 8 * KB, 8 * KB)]
        junk2 = cv(A3 + 28 * KB, 4 * KB, BF16)
        b_p9c, b_h2f, b_h2Tf, b_junk2 = Buf("p9c"), Buf("h2f"), Buf("h2Tf"), Buf("junk2")
        alias([b_p9c, b_h2f, b_h2Tf, b_junk2], b_mT)
        b_h2f_l = [b_h2f, Buf("h2f1")]
        b_h2Tf_l = [[b_h2Tf, Buf("h2Tf0b")], [Buf("h2Tf1a"), Buf("h2Tf1b")]]
        alias([b_h2f_l[1]] + b_h2Tf_l[1], [b_bt, b_off, b_st, b_st2[1], b_g1B] + b_g7 + b_sgS[0] + b_sgS[1] + b_sgF[0] + b_sgF[1] + b_PT + b_rinv2 + b_tsq + b_trt)
        alias([b_h2Tf_l[0][1]], b_mT)
        S.dma("sp", lambda e: e.dma_start(out=g2B, in_=g2B_d), writes=[b_p9c], semkey="c_g2B")
        S.dma("sp", lambda e: e.dma_start(out=wr3, in_=wr_d.rearrange("(k p) n -> p k n", p=128)), writes=[b_p9c], semkey="c_wr")
        S.dma("sp", lambda e: e.dma_start(out=brB, in_=brB_d), writes=[b_p9c], semkey="c_brB")
        Lg = cv(TR, 1152).rearrange("p (i n) -> p i n", i=8)
        posm = cv(TR + 1280, 1024).rearrange("p (i n) -> p i n", i=8)
        gate = cv(TR + 2304, 1024).rearrange("p (i n) -> p i n", i=8)
        Mm = cv(TR + 3328, 1024).rearrange("p (i n) -> p i n", i=8)
        scr = [cv(TR + 4352 + i * 1024, 1024) for i in range(2)]
        b_L, b_tab, b_M = Buf("L"), Buf("tab"), Buf("M")
        b_scr = [Buf(), Buf()]
        alias([b_L, b_tab, b_M] + b_scr, [b_h2f_l[1]] + b_h2Tf_l[1] + b_g7 + b_sgS[0] + b_sgS[1] + b_sgF[0] + b_sgF[1])

        def p9_tile(i):
            S.op("act", lambda e: e.activation(out=junk2, in_=x1[:, i, :], func=AF.Square, accum_out=ssv[:, i:i + 1]), reads=[b_x1[i]], writes=[b_junk2, b_ssl[i]])
            S.op("act", lambda e: e.activation(out=rtv[:, i:i + 1], in_=ssv[:, i:i + 1], func=AF.Sqrt, scale=1.0 / 2048, bias=epsc), reads=[b_ssl[i], b_const], writes=[b_ssl[i]])
            S.op("dve", lambda e: e.reciprocal(rstdv[:, i:i + 1], rtv[:, i:i + 1]), reads=[b_ssl[i]], writes=[b_ssl[i]])
            h2f, h2Tf = h2f_l[i % 2], h2Tf_l[i % 2]
            bh2f, bh2Tf = b_h2f_l[i % 2], b_h2Tf_l[i % 2]
            S.op("dve", lambda e: e.scalar_tensor_tensor(out=h2f, in0=x1[:, i, :], scalar=rstdv[:, i:i + 1], in1=g2B, op0=ALU.mult, op1=ALU.mult),
                 reads=[b_x1[i], b_ssl[i], b_p9c], writes=[bh2f])
            S.op("act", lambda e: e.activation(out=h2[:, i, :], in_=h2f, func=AF.Copy), reads=[bh2f], writes=[b_h2[i]])
            psT = psf(0, 4)
            for k in range(16):
                S.op("pe", (lambda e, k=k: e.transpose(psT[:, k * 128:(k + 1) * 128], h2f[:, k * 128:(k + 1) * 128], ident_f)),
                     reads=[bh2f, b_const], writes=[bps[k // 4]])
            S.op("act", lambda e: e.activation(out=h2Tf[:, 0:1024], in_=psT[:, 0:1024], func=AF.Copy), reads=[bps[0], bps[1]], writes=[bh2Tf[0]])
            S.op("dve", lambda e: e.tensor_copy(h2Tf[:, 1024:2048], psT[:, 1024:2048]), reads=[bps[2], bps[3]], writes=[bh2Tf[1]])
            bl = 4 + i % 2
            h2T3 = h2Tf.rearrange("p (k t) -> p k t", k=16)
            for k in range(16):
                S.op("pe", (lambda e, k=k: e.matmul(ps[:, bl, 0:36], h2T3[:, k, :], wr3[:, k, :], start=(k == 0), stop=(k == 15))),
                     reads=[bh2Tf[k // 8], b_p9c], writes=[bps[bl]])
            S.op("dve", lambda e: e.tensor_tensor(out=Lg[:, i, :], in0=ps[:, bl, 0:36], in1=brB, op=ALU.add), reads=[bps[bl], b_p9c], writes=[b_L])

        for i in range(8):
            p9_tile(i)

        alias(b_scr, [b_h2f_l[1]] + b_h2Tf_l[1])
        rs = TR + 6400
        def rsc(nfl):
            nonlocal_rs[0] += nfl * 4
            return cv(nonlocal_rs[0] - nfl * 4, nfl * 4)
        nonlocal_rs = [rs]
        r_gmax, r_gsum, r_pg, r_m1, r_m2, r_d, r_ed, r_den, r_w1, r_g1, r_g2 = [rsc(8) for _ in range(11)]
        r_goh, r_gsh, r_pen = [rsc(32).rearrange("p (i g) -> p i g", i=8) for _ in range(3)]
        r_em, r_oh1, r_em2, r_oh2 = [rsc(256).rearrange("p (i n) -> p i n", i=8) for _ in range(4)]
        assert nonlocal_rs[0] <= TR + 12 * KB
        bs = b_scr[0]
        gl3 = Lg[:, :, 0:4]
        el4 = Lg[:, :, 4:36].rearrange("p i (g e) -> p i g e", g=4)

        def bc8(ap, n):
            return ap.unsqueeze(2).broadcast_to([128, 8, n])

        def D(fn, reads=(), writes=()):
            S.op("dve", fn, reads=[bs] + list(reads), writes=[bs] + list(writes))

        fl = lambda ap: ap.rearrange("p i n -> p (i n)")
        D(lambda e: e.tensor_reduce(out=r_gmax, in_=gl3, axis=AX.X, op=ALU.max), reads=[b_L])
        D(lambda e: e.tensor_tensor(out=r_goh, in0=gl3, in1=bc8(r_gmax, 4), op=ALU.is_ge), reads=[b_L])
        D(lambda e: e.tensor_tensor(out=r_gsh, in0=gl3, in1=bc8(r_gmax, 4), op=ALU.subtract), reads=[b_L])
        S.op("act", lambda e: e.activation(out=fl(r_gsh), in_=fl(r_gsh), func=AF.Exp), reads=[bs], writes=[bs])
        D(lambda e: e.tensor_reduce(out=r_gsum, in_=r_gsh, axis=AX.X, op=ALU.add))
        D(lambda e: e.reciprocal(r_pg, r_gsum))
        D(lambda e: e.tensor_scalar(fl(r_pen), fl(r_goh), 1.0, BIG, ALU.subtract, ALU.mult))
        D(lambda e: e.tensor_tensor(out=r_em.rearrange("p i (g e) -> p i g e", g=4), in0=el4, in1=r_pen.unsqueeze(3).broadcast_to([128, 8, 4, 8]), op=ALU.add), reads=[b_L])
        D(lambda e: e.tensor_reduce(out=r_m1, in_=r_em, axis=AX.X, op=ALU.max))
        D(lambda e: e.tensor_tensor(out=r_oh1, in0=r_em, in1=bc8(r_m1, 32), op=ALU.is_ge))
        D(lambda e: e.scalar_tensor_tensor(out=fl(r_em2), in0=fl(r_oh1), scalar=-BIG, in1=fl(r_em), op0=ALU.mult, op1=ALU.add))
        D(lambda e: e.tensor_reduce(out=r_m2, in_=r_em2, axis=AX.X, op=ALU.max))
        D(lambda e: e.tensor_tensor(out=r_oh2, in0=r_em2, in1=bc8(r_m2, 32), op=ALU.is_ge))
        D(lambda e: e.tensor_tensor(out=r_d, in0=r_m2, in1=r_m1, op=ALU.subtract))
        S.op("act", lambda e: e.activation(out=r_ed, in_=r_d, func=AF.Exp), reads=[bs], writes=[bs])
        D(lambda e: e.tensor_scalar(r_den, r_ed, 1.0, None, ALU.add))
        D(lambda e: e.reciprocal(r_w1, r_den))
        D(lambda e: e.tensor_tensor(out=r_g1, in0=r_w1, in1=r_pg, op=ALU.mult))
        D(lambda e: e.tensor_tensor(out=r_g2, in0=r_g1, in1=r_ed, op=ALU.mult))
        D(lambda e: e.tensor_tensor(out=gate, in0=r_oh1, in1=bc8(r_g1, 32), op=ALU.mult), writes=[b_tab])
        D(lambda e: e.tensor_tensor(out=r_em, in0=r_oh2, in1=bc8(r_g2, 32), op=ALU.mult))
        D(lambda e: e.tensor_tensor(out=fl(gate), in0=fl(gate), in1=fl(r_em), op=ALU.add), reads=[b_tab], writes=[b_tab])
        D(lambda e: e.tensor_tensor(out=fl(Mm), in0=fl(r_oh1), in1=fl(r_oh2), op=ALU.add), writes=[b_M])
        for i in range(8):
            for i2 in range(i + 1):
                S.op("pe", (lambda e, i=i, i2=i2: e.matmul(ps[:, 6, i * 32:(i + 1) * 32], (triS_f if i2 == i else ones_f), Mm[:, i2, :], start=(i2 == 0), stop=(i2 == i))),
                     reads=[b_M, b_const], writes=[bps[6]])
        S.op("dve", lambda e: e.tensor_tensor(out=posm.rearrange("p i n -> p (i n)"), in0=ps[:, 6, 0:256], in1=Mm.rearrange("p i n -> p (i n)"), op=ALU.mult),
             reads=[bps[6], b_M], writes=[b_tab])
        S.op("dve", lambda e: e.scalar_tensor_tensor(out=posm.rearrange("p i n -> p (i n)"), in0=Mm.rearrange("p i n -> p (i n)"), scalar=-1.0,
                                                     in1=posm.rearrange("p i n -> p (i n)"), op0=ALU.add, op1=ALU.add),
             reads=[b_M, b_tab], writes=[b_tab])
        dump("L", Lg, [b_L])
        dump("posm", posm, [b_tab])
        dump("gate", gate, [b_tab])
        if upto <= 10:
            return finish(nc, S, dumps, dbg_d)

        Yb = [cv(A3 + q * 4 * KB, 4 * KB, BF16) for q in range(4)]
        SgT = [cv(A3 + 16 * KB + q * 2 * KB, 2 * KB, BF16).rearrange("p (j t) -> p j t", j=8) for q in range(8)]
        b_Yb2 = [[Buf(), Buf()] for _ in range(4)]
        b_Yb = [b for r in b_Yb2 for b in r]
        b_SgT = [Buf() for _ in range(8)]
        alias(b_Yb + b_SgT, [b_p9c, b_h2f, b_h2Tf, b_junk2, b_h2Tf_l[0][1]])
        sil = cv(TR + 4352, 2 * KB)
        T2 = TR + 6400
        Sm = cv(T2, 2 * KB, BF16).rearrange("p (j s) -> p j s", j=8)
        Sg = cv(T2 + 2 * KB, 2 * KB, BF16).rearrange("p (j s) -> p j s", j=8)
        AT = [cv(T2 + 4 * KB + i * KB, KB, BF16).rearrange("p (f s) -> p f s", f=4) for i in range(2)]
        Aa = [cv(T2 + 6 * KB + i * KB, KB, BF16) for i in range(2)]
        XeT = [cv(T2 + 8 * KB + i * 4 * KB, 4 * KB, BF16).rearrange("p (c s) -> p c s", c=16) for i in range(2)]
        assert T2 + 16 * KB <= 207 * KB
        b_Sm = [Buf("Sm%d" % j) for j in range(8)]
        b_Sg = [Buf("Sg%d" % j) for j in range(8)]
        b_sil = Buf("sil")
        b_AT = [Buf(), Buf()]
        b_Aa = [Buf(), Buf()]
        b_XeT2 = [[Buf(), Buf()] for _ in range(2)]
        b_XeT = [b for r in b_XeT2 for b in r]
        alias(b_Sm + b_Sg + [b_sil] + b_AT + b_Aa + b_XeT, [b_bt, b_off, b_st, b_st2[1], b_h2f_l[1]] + b_h2Tf_l[1] + b_g7 + b_sgS[0] + b_sgS[1] + b_sgF[0] + b_sgF[1] + b_PT + b_rinv2 + b_scr)
        urot = {"i": 0}

        def unit2():
            u = urot["i"] % 2
            urot["i"] += 1
            return 2 * u

        def onehots(ex):
            for j in range(8):
                S.op("dve", (lambda e, j=j: e.tensor_scalar(Sm[:, j, :], iota_f, posm[:, j, ex:ex + 1], None, ALU.is_equal)),
                     reads=[b_tab, b_const], writes=[b_Sm[j]])
            for j in range(8):
                S.op("dve", (lambda e, j=j: e.tensor_scalar(Sg[:, j, :], iota_f, posm[:, j, ex:ex + 1], gate[:, j, ex:ex + 1], ALU.is_equal, ALU.mult)),
                     reads=[b_tab, b_const], writes=[b_Sg[j]])

        def sgt_tr(ex):
            qq = ex % 8
            psTr = ps[:, 6:7, :].bitcast(BF16).rearrange("p a (j t) -> p (a j) t", t=128)
            for j in range(8):
                S.op("pe", (lambda e, j=j: e.transpose(psTr[:, j, :], Sg[:, j, :], ident_bf)), reads=[b_Sg[j], b_const], writes=[bps[6]])
            evac(SgT[qq], psTr, [bps[6]], [b_SgT[qq]], eng="act")

        def gather_half(ex, hf):
            sl = ex % 2
            b0 = unit2()
            psG = psf(b0, 2)
            for cc in range(8):
                c_ = hf * 8 + cc
                for j in range(8):
                    S.op("pe", (lambda e, cc=cc, c_=c_, j=j: e.matmul(psG[:, cc * 128:(cc + 1) * 128], h2[:, j, c_ * 128:(c_ + 1) * 128], Sm[:, j, :],
                                                                      start=(j == 0), stop=(j == 7))),
                         reads=[b_h2[j], b_Sm[j]], writes=[bps[b0 + cc // 4]])
            evac(XeT[sl][:, hf * 8:(hf + 1) * 8, :], psG.rearrange("p (c s) -> p c s", c=8), [bps[b0], bps[b0 + 1]], [b_XeT2[sl][hf]], eng="act")

        def gateup(ex):
            sl = ex % 2
            for bank in (4, 5):
                for half in range(2):
                    w, b_w = ws_take()
                    w3 = w.rearrange("p (k n) -> p k n", k=8)
                    for k in range(8):
                        c_ = half * 8 + k
                        S.op("pe", (lambda e, k=k, c_=c_, w3=w3, bank=bank: e.matmul(ps[:, bank, :], XeT[sl][:, c_, :], w3[:, k, :], start=(c_ == 0), stop=(c_ == 15))),
                             reads=[b_w, b_XeT2[sl][half]], writes=[bps[bank]])
                    ws_release()
            S.op("act", lambda e: e.activation(out=sil, in_=ps[:, 4, :], func=AF.Silu), reads=[bps[4]], writes=[b_sil])
            S.op("dve", lambda e: e.tensor_tensor(out=Aa[sl], in0=sil, in1=ps[:, 5, :], op=ALU.mult), reads=[b_sil, bps[5]], writes=[b_Aa[sl]])

        def a_tr(ex):
            sl = ex % 2
            psA = ps[:, 7:8, 0:256].bitcast(BF16).rearrange("p a (f s) -> p (a f) s", s=128)
            for fc in range(4):
                S.op("pe", (lambda e, fc=fc: e.transpose(psA[:, fc, :], Aa[sl][:, fc * 128:(fc + 1) * 128], ident_bf)), reads=[b_Aa[sl], b_const], writes=[bps[7]])
            evac(AT[sl], psA, [bps[7]], [b_AT[sl]], eng="act")

        def down(ex):
            sl = ex % 2
            q = ex % 4
            wlo, b_wlo = ws_take()
            whi, b_whi = ws_take()
            wl3 = wlo.rearrange("p (k n) -> p k n", k=2)
            wh3 = whi.rearrange("p (k n) -> p k n", k=2)
            for hf in range(2):
                b0 = unit2()
                for fc in range(4):
                    wsrc, bw = (wl3, b_wlo) if fc < 2 else (wh3, b_whi)
                    for nn in range(2):
                        n = hf * 2 + nn
                        S.op("pe", (lambda e, nn=nn, n=n, fc=fc, b0=b0, wsrc=wsrc: e.matmul(ps[:, b0 + nn, :], AT[sl][:, fc, :], wsrc[:, fc % 2, n * 512:(n + 1) * 512],
                                                                                            start=(fc == 0), stop=(fc == 3))),
                             reads=[bw, b_AT[sl]], writes=[bps[b0 + nn]])
                evac(Yb[q][:, hf * 1024:(hf + 1) * 1024], psf(b0, 2), [bps[b0], bps[b0 + 1]], [b_Yb2[q][hf]], eng="act")
            ws_release()
            ws_release()

        def combine(quad):
            for j in range(8):
                for hf in range(2):
                    b0 = unit2()
                    for nn in range(2):
                        n = hf * 2 + nn
                        for q in range(4):
                            qq = (quad % 2) * 4 + q
                            S.op("pe", (lambda e, nn=nn, n=n, q=q, qq=qq, b0=b0, j=j: e.matmul(ps[:, b0 + nn, :], SgT[qq][:, j, :], Yb[q][:, n * 512:(n + 1) * 512], start=(q == 0), stop=(q == 3))),
                                 reads=[b_SgT[qq], b_Yb2[q][hf]], writes=[bps[b0 + nn]])
                    S.op("dve", (lambda e, hf=hf, b0=b0, j=j: e.tensor_tensor(out=x1[:, j, hf * 1024:(hf + 1) * 1024], in0=x1[:, j, hf * 1024:(hf + 1) * 1024], in1=psf(b0, 2), op=ALU.add)),
                         reads=[bps[b0], bps[b0 + 1], b_x1[j]], writes=[b_x1[j]])

        NEXP = 32 if upto > 11 else 4
        onehots(0)
        sgt_tr(0)
        gather_half(0, 0)
        gather_half(0, 1)
        for s_ in range(NEXP):
            if s_ + 1 < NEXP:
                onehots(s_ + 1)
            gateup(s_)
            if s_ + 1 < NEXP:
                sgt_tr(s_ + 1)
                gather_half(s_ + 1, 0)
            a_tr(s_)
            if s_ + 1 < NEXP:
                gather_half(s_ + 1, 1)
            down(s_)
            if s_ % 4 == 3:
                combine(s_ // 4)
        dump("x1f", x1, b_x1)

        b_out = [Buf("out%d" % i) for i in range(8)]
        for i in range(8):
            S.dma("sp", (lambda e, i=i: e.dma_start(out=out_d[i * 128:(i + 1) * 128, :], in_=x1[:, i, :])), reads=[b_x1[i]], writes=[b_out[i]], semkey=("out", i))
        S.final_wait("sp", b_out)
        return finish(nc, S, dumps, dbg_d)
    return nc


def finish(nc, S, dumps, dbg_d):
    allb = []
    for name, ap, bufs in dumps:
        S.dma("pool", (lambda e, name=name, ap=ap: e.dma_start(out=dbg_d[name], in_=ap)), reads=bufs, writes=bufs, semkey="dump_" + name)
        allb.extend(bufs)
    S.final_wait("pool", allb)
    S.run()
    return nc


def prep_inputs(x, norm1_g, w_in, b_gate, b_forget, sgu_ln_g, sgu_ln_b, w_spatial, b_spatial,
                q_norm_g, k_norm_g, w_proj_sgu, w_proj_fox, w_out, norm2_g,
                w_router_group, b_router_group, w_router_expert, b_router_expert,
                w_expert_gate, w_expert_up, w_expert_down):
    f = lambda a: np.ascontiguousarray(np.asarray(a, dtype=np.float32))
    x = f(x)
    rep = lambda v, n=128: np.ascontiguousarray(np.broadcast_to(f(v).reshape(1, -1), (n, f(v).size)))
    shared = {
        "g1B": rep(norm1_g[0]),
        "g2B": rep(norm2_g[0]),
        "w_in": f(w_in[0]),
        "bgT": np.ascontiguousarray(f(b_gate[0]).reshape(32, 128).T),
        "bfB": rep(np.tile(f(b_forget[0]), 16)),
        "lngB": rep(sgu_ln_g[0]),
        "lnbB": rep(sgu_ln_b[0]),
        "wsT": np.ascontiguousarray(f(w_spatial[0]).transpose(2, 0, 1).reshape(128, 1024)),
        "bspB": rep(f(b_spatial[0]).reshape(-1)),
        "qg": f(q_norm_g[0]).reshape(128, 1),
        "kg": f(k_norm_g[0]).reshape(128, 1),
        "wps": f(w_proj_sgu[0]),
        "wpf": f(w_proj_fox[0]),
        "wout": f(w_out[0]),
        "wr": np.ascontiguousarray(np.concatenate([f(w_router_group[0]), f(w_router_expert[0])], axis=1)),
        "brB": rep(np.concatenate([f(b_router_group[0]), f(b_router_expert[0])])),
        "weg": f(w_expert_gate[0]),
        "weu": f(w_expert_up[0]),
        "wed": f(w_expert_down[0]),
    }
    zeros = np.zeros((1024, 2048), np.float32)
    in_maps = []
    for c in range(8):
        b, half = c // 2, c % 2
        m = dict(shared)
        m["xo"] = np.ascontiguousarray(x[b, half * 1024:(half + 1) * 1024])
        m["xp"] = np.ascontiguousarray(x[b, 0:1024]) if half == 1 else zeros
        m["maskb"] = np.full((128, 1), 0.0 if half == 1 else -30000.0, np.float32)
        in_maps.append(m)
    return in_maps


_NC = {}


def kernel(**inputs):
    in_maps = prep_inputs(**inputs)
    if "nc" not in _NC:
        _NC["nc"] = build()
    res = run_bass_kernel_spmd(_NC["nc"], in_maps, core_ids=list(range(8)))
    out = np.zeros((4, 2048, 2048), np.float32)
    for c in range(8):
        b, half = c // 2, c % 2
        out[b, half * 1024:(half + 1) * 1024] = res.results[c]["out"]
    return out
```

```python
import numpy as np
import concourse.bass as bass
import concourse.mybir as mybir
from concourse.alu_op_type import AluOpType as ALU
from concourse.bass_utils import run_bass_kernel_spmd

F32 = mybir.dt.float32
BF16 = mybir.dt.bfloat16
AF = mybir.ActivationFunctionType
AX = mybir.AxisListType
ENGS = ("pe", "act", "dve", "pool", "sp")
KB = 1024
EPS = 1e-6
SCALE = 128 ** -0.5
BIG = 1.0e4


class Buf:
    __slots__ = ("name", "w", "r")

    def __init__(self, name=""):
        self.name = name
        self.w = None
        self.r = []


def alias(new_bufs, old_bufs):
    evs = []
    for o in old_bufs:
        if o.w is not None:
            evs.append(o.w)
        evs.extend(o.r)
    red = {}
    for k, v in evs:
        if red.get(k, 0) < v:
            red[k] = v
    evs = list(red.items())
    for n in new_bufs:
        n.r = list(n.r) + evs


class Sched:
    def __init__(self, nc):
        self.nc = nc
        self.prog = {e: [] for e in ENGS}
        self.cnt = {}
        self.waited = {e: {} for e in ENGS}
        self.sems = {}
        self.same_engine_sync = {"act": True, "dve": True, "pool": True, "pe": False, "sp": False}

    def sem(self, key):
        if key not in self.sems:
            self.sems[key] = self.nc.alloc_semaphore(name="s%d" % len(self.sems))
            self.cnt[key] = 0
        return self.sems[key]

    def _deps(self, eng, reads, writes):
        deps = {}

        def add(ev):
            if ev is None:
                return
            k, v = ev
            if deps.get(k, 0) < v:
                deps[k] = v

        for b in reads:
            add(b.w)
        for b in writes:
            add(b.w)
            for r in b.r:
                add(r)
        waits = []
        for k, v in deps.items():
            if k == eng and not self.same_engine_sync.get(eng, True):
                continue
            if self.waited[eng].get(k, 0) >= v:
                continue
            self.waited[eng][k] = v
            waits.append((k, v))
        return waits

    def op(self, eng, fn, reads=(), writes=()):
        self.sem(eng)
        waits = self._deps(eng, reads, writes)
        self.cnt[eng] += 1
        ev = (eng, self.cnt[eng])
        self.prog[eng].append((waits, fn, (eng, 1)))
        for b in reads:
            b.r.append(ev)
        for b in writes:
            b.w = ev
            b.r = []
        return ev

    def dma(self, queue, fn, reads=(), writes=(), semkey=None, chain=False):
        self.sem(semkey)
        waits = [] if chain else self._deps(queue, reads, writes)
        self.cnt[semkey] += 16
        ev = (semkey, self.cnt[semkey])
        self.prog[queue].append((waits, fn, (semkey, 16)))
        for b in reads:
            b.r.append(ev)
        for b in writes:
            b.w = ev
            b.r = []
        return ev

    def final_wait(self, queue, bufs):
        waits = self._deps(queue, bufs, ())
        self.prog[queue].append((waits, None, None))

    def emit(self, eng, h):
        for waits, fn, inc in self.prog[eng]:
            for k, v in waits:
                h.wait_ge(self.sems[k], v)
            if fn is None:
                continue
            ins = fn(h)
            ins.then_inc(self.sems[inc[0]], inc[1])

    def run(self):
        nc = self.nc
        with nc.Block() as block:
            @block.tensor
            def _(e):
                self.emit("pe", e)

            @block.scalar
            def _(e):
                self.emit("act", e)

            @block.vector
            def _(e):
                self.emit("dve", e)

            @block.gpsimd
            def _(e):
                self.emit("pool", e)

            @block.sync
            def _(e):
                self.emit("sp", e)


OFF_U, OFF_V, OFF_Q, OFF_K, OFF_VA, OFF_F, OFF_GATE = 0, 1024, 2048, 3072, 4096, 5120, 5128
CONV_EXPERTS = 3
IN_COLS = 9224


def build(dbg=(), upto=99):
    nc = bass.Bass("TRN2", target_bir_lowering=False)

    def din(name, shape):
        return nc.dram_tensor(name, list(shape), F32, kind="ExternalInput").ap()

    xo = din("xo", [1024, 2048])
    xp = din("xp", [1024, 2048])
    maskb_d = din("maskb", [128, 1])
    g1B_d = din("g1B", [128, 2048])
    g2B_d = din("g2B", [128, 2048])
    w_in = din("w_in", [2048, IN_COLS])
    bgT_d = din("bgT", [128, 32])
    bfB_d = din("bfB", [128, 128])
    lngB_d = din("lngB", [128, 1024])
    lnbB_d = din("lnbB", [128, 1024])
    wsT_d = din("wsT", [128, 1024])
    bspB_d = din("bspB", [128, 1024])
    qg_d = din("qg", [128, 1])
    kg_d = din("kg", [128, 1])
    wps_d = din("wps", [1024, 2048])
    wpf_d = din("wpf", [1024, 2048])
    wout_d = din("wout", [2048, 2048])
    wr_d = din("wr", [2048, 36])
    brB_d = din("brB", [128, 36])
    weg_d = din("weg", [32, 2048, 512])
    weu_d = din("weu", [32, 2048, 512])
    wed_d = din("wed", [32, 512, 2048])
    out_d = nc.dram_tensor("out", [1024, 2048], F32, kind="ExternalOutput").ap()
    dbg_d = {}
    for name, shape in dbg:
        dbg_d[name] = nc.dram_tensor("dbg_" + name, list(shape), F32, kind="ExternalOutput").ap()

    NCONV_E = CONV_EXPERTS
    if NCONV_E > 0:
        wsc_g = nc.dram_tensor("wsc_g", [NCONV_E, 2048, 512], BF16, kind="Internal").ap()
        wsc_u = nc.dram_tensor("wsc_u", [NCONV_E, 2048, 512], BF16, kind="Internal").ap()
        wsc_d = nc.dram_tensor("wsc_d", [NCONV_E, 512, 2048], BF16, kind="Internal").ap()
    S = Sched(nc)
    arena_cm = nc.sbuf_tensor("arena", [128, 207 * KB // 4], F32)
    ps_cm = nc.psum_tensor("ps", [128, 8, 512], F32)
    with arena_cm as arena, ps_cm as ps:
        def cv(off, nbytes, dt=F32):
            a = arena[:, off // 4:(off + nbytes) // 4]
            return a if dt == F32 else a.bitcast(dt)

        A0, A1, A2, A3, RING, CR, TR = 0, 32 * KB, 64 * KB, 96 * KB, 128 * KB, 176 * KB, 184 * KB
        bps = [Buf("ps%d" % i) for i in range(8)]

        def psf(b0, n=1):
            v = ps[:, b0:b0 + n, :]
            return v.rearrange("p a n -> p (a n)")

        c = CR
        ident_bf = cv(c, 256, BF16); c += 256
        negmask_bf = cv(c, 256, BF16); c += 256
        ones_bf = cv(c, 256, BF16); c += 256
        ident_f = cv(c, 512); c += 512
        triU_f = cv(c, 512); c += 512
        triS_f = cv(c, 512); c += 512
        ones_f = cv(c, 512); c += 512
        iota_f = cv(c, 512); c += 512
        bgT = cv(c, 128); c += 128
        bfB = cv(c, 512); c += 512
        smalls = cv(c, 32); c += 32
        pidx, qg, kg, maskb, epsc, zeroc, onec = [smalls[:, i:i + 1] for i in range(7)]
        wf = cv(c, 256, BF16).rearrange("p (k n) -> p k n", k=16); c += 256
        fz = cv(c, 512); c += 512
        spt = cv(c, 512); c += 512
        Cc = cv(c, 512); c += 512
        pre = cv(c, 512); c += 512
        Cm = cv(c, 512); c += 512
        offT = cv(TR + 12 * KB, 2 * KB, BF16)
        selAll = cv(TR + 14 * KB, 2 * KB, BF16).rearrange("p (h j) -> p h j", h=8)
        iotaH = cv(TR + 16 * KB, 4 * KB)
        ssv = cv(c, 64); c += 64
        rtv = cv(c, 64); c += 64
        rstdv = cv(c, 64); c += 64
        assert c <= CR + 8 * KB, c - CR
        b_const = Buf("const")
        b_iota = Buf("iota")

        def ld_const(dst, src, key):
            S.dma("sp", lambda e: e.dma_start(out=dst, in_=src), writes=[b_const], semkey="c_" + key)

        S.op("pool", lambda e: e.iota(iota_f, [[1, 128]], base=0, channel_multiplier=0, allow_small_or_imprecise_dtypes=True), writes=[b_iota])
        S.op("pool", lambda e: e.iota(pidx, [[0, 1]], base=0, channel_multiplier=1, allow_small_or_imprecise_dtypes=True), writes=[b_iota])
        ld_const(qg, qg_d, "qg")
        ld_const(kg, kg_d, "kg")
        ld_const(maskb, maskb_d, "maskb")
        ld_const(bgT, bgT_d, "bgT")
        ld_const(bfB, bfB_d, "bfB")
        S.op("dve", lambda e: e.memset(epsc, EPS), reads=[b_iota], writes=[b_const])
        S.op("dve", lambda e: e.memset(zeroc, 0.0), writes=[b_const])
        S.op("dve", lambda e: e.memset(onec, 1.0), writes=[b_const])
        S.op("dve", lambda e: e.memset(ones_f, 1.0), writes=[b_const])
        S.op("dve", lambda e: e.memset(ones_bf, 1.0), writes=[b_const])
        S.op("dve", lambda e: e.tensor_scalar(ident_f, iota_f, pidx, None, ALU.is_equal), reads=[b_iota], writes=[b_const])
        S.op("dve", lambda e: e.tensor_scalar(ident_bf, iota_f, pidx, None, ALU.is_equal), reads=[b_iota], writes=[b_const])
        S.op("dve", lambda e: e.tensor_scalar(triU_f, iota_f, pidx, None, ALU.is_ge), reads=[b_iota], writes=[b_const])
        S.op("dve", lambda e: e.tensor_scalar(negmask_bf, iota_f, pidx, -30000.0, ALU.is_lt, ALU.mult), reads=[b_iota], writes=[b_const])
        S.op("dve", lambda e: e.tensor_scalar(triS_f, iota_f, pidx, None, ALU.is_gt), reads=[b_iota], writes=[b_const])
        S.dma("pool", lambda e: e.dma_start(out=wf, in_=w_in[:, OFF_F:OFF_F + 8].rearrange("(k p) n -> p k n", p=128)),
              writes=[b_const], semkey="c_wf")

        NS8 = 6
        b8 = [Buf("r8_%d" % i) for i in range(NS8)]
        b16 = [Buf("r16_%d" % i) for i in range(NS8 // 2)]
        ap8 = [cv(RING + i * 8 * KB, 8 * KB, BF16) for i in range(NS8)]
        ap16 = [cv(RING + i * 16 * KB, 16 * KB, BF16) for i in range(NS8 // 2)]
        units = []

        def u_cols(src, c0, ncols, K):
            parts = []
            for k0 in range(0, K, 4):
                parts.append((lambda s, k0=k0, K=K, ncols=ncols: s[:, 0:K * ncols].rearrange("p (k n) -> p k n", k=K)[:, k0:k0 + 4, :],
                              src[k0 * 128:(k0 + 4) * 128, c0:c0 + ncols].rearrange("(k p) n -> p k n", p=128)))
            return (2, parts)

        for blk in range(2):
            units.append(u_cols(w_in, OFF_K + blk * 512, 512, 16))
        for blk in range(2):
            units.append(u_cols(w_in, OFF_VA + blk * 512, 512, 16))
        for blk in range(2):
            units.append(u_cols(w_in, OFF_Q + blk * 512, 512, 16))
        for blk in range(2):
            units.append(u_cols(w_in, OFF_U + blk * 512, 512, 16))
        for blk in range(2):
            units.append(u_cols(w_in, OFF_V + blk * 512, 512, 16))
        for n in range(4):
            units.append(u_cols(w_in, OFF_GATE + n * 512, 512, 16))
            units.append(u_cols(w_in, OFF_GATE + 2048 + n * 512, 512, 16))
            pp = []
            for half, srcw in enumerate((wps_d, wpf_d)):
                for k0 in (0, 4):
                    pp.append((lambda s, half=half, k0=k0: s.rearrange("p (k n) -> p k n", k=16)[:, half * 8 + k0:half * 8 + k0 + 4, :],
                               srcw[k0 * 128:(k0 + 4) * 128, n * 512:(n + 1) * 512].rearrange("(k p) n -> p k n", p=128)))
            units.append((2, pp))
        for n in range(4):
            units.append(u_cols(wout_d, n * 512, 512, 16))
        b_wsc = [Buf("wsc%d" % ex) for ex in range(32)]
        conv = []
        for ex in range(NCONV_E):
            for srcw, dstw, rows in ((weg_d, wsc_g, 2048), (weu_d, wsc_u, 2048), (wed_d, wsc_d, 512)):
                step = rows // 4
                for r0 in range(0, rows, step):
                    conv.append((ex, dstw[ex][r0:r0 + step, :], srcw[ex][r0:r0 + step, :]))
        unit_ex = {}
        for ex in range(32):
            cvt = ex < NCONV_E
            for wi, wsrc in enumerate((weg_d, weu_d)):
                srcm = ((wsc_g, wsc_u)[wi][ex]) if cvt else wsrc[ex]
                for half in range(2):
                    pp = []
                    for k0 in (0, 4):
                        r0 = half * 1024 + k0 * 128
                        pp.append((lambda s, k0=k0: s.rearrange("p (k n) -> p k n", k=8)[:, k0:k0 + 4, :],
                                   srcm[r0:r0 + 512, :].rearrange("(k p) n -> p k n", p=128)))
                    unit_ex[len(units)] = ex
                    units.append((1, pp))
            srcm = wsc_d[ex] if cvt else wed_d[ex]
            for half in range(2):
                pp = []
                for k0 in range(2):
                    r0 = half * 256 + k0 * 128
                    pp.append((lambda s, k0=k0: s.rearrange("p (k n) -> p k n", k=2)[:, k0:k0 + 1, :],
                               srcm[r0:r0 + 128, :].rearrange("(k p) n -> p k n", p=128)))
                unit_ex[len(units)] = ex
                units.append((1, pp))
        cvs = {"done": False}

        def conv_burst():
            for ex, dst, src in conv:
                S.dma("pool", (lambda e, dst=dst, src=src: e.dma_start(out=dst, in_=src)), writes=[], semkey=("conv", ex), chain=True)
                b_wsc[ex].w = (("conv", ex), S.cnt[("conv", ex)])
            cvs["done"] = True

        ws = {"iu": 0, "tu": 0, "ipos": 0, "tpos": 0, "upos": {}}

        def ws_issue():
            while ws["iu"] < len(units):
                size, parts = units[ws["iu"]]
                if ws["ipos"] + size - ws["tpos"] > NS8:
                    return
                p = ws["ipos"] % NS8
                rd = []
                if ws["iu"] in unit_ex and unit_ex[ws["iu"]] < NCONV_E:
                    assert cvs["done"], "conversion must be queued before the loads that read it"
                    rd = [b_wsc[unit_ex[ws["iu"]]]]
                if size == 2:
                    assert p % 2 == 0
                    dst_ap, wb, key = ap16[p // 2], [b16[p // 2], b8[p], b8[p + 1]], ("ring16", p // 2)
                else:
                    alias([b8[p]], [b16[p // 2]])
                    dst_ap, wb, key = ap8[p], [b8[p]], ("ring8", p)
                for pi, (dst_fn, src) in enumerate(parts):
                    dst = dst_fn(dst_ap)
                    S.dma("pool", (lambda e, dst=dst, src=src: e.dma_start(out=dst, in_=src)), reads=rd, writes=wb, semkey=key, chain=(pi > 0))
                ws["upos"][ws["iu"]] = p
                ws["iu"] += 1
                ws["ipos"] += size

        def ws_take():
            u = ws["tu"]
            assert u < ws["iu"], "weight unit not issued yet"
            size, _ = units[u]
            p = ws["upos"][u]
            ws["tu"] += 1
            ws["pending_release"] = ws.get("pending_release", [])
            ws["pending_release"].append(size)
            if size == 2:
                return ap16[p // 2], b16[p // 2]
            return ap8[p], b8[p]

        def ws_release():
            size = ws["pending_release"].pop(0)
            ws["tpos"] += size
            ws_issue()

        ws_issue()

        hTp = cv(A0, 32 * KB, BF16).rearrange("p (k t) -> p k t", k=16)
        hTo = cv(A1, 32 * KB, BF16).rearrange("p (k t) -> p k t", k=16)

        def hTs(k, t0, n):
            return hTp[:, k, t0:t0 + n] if t0 < 1024 else hTo[:, k, t0 - 1024:t0 - 1024 + n]

        def hTall(i):
            return hTp[:, :, i * 128:(i + 1) * 128] if i < 8 else hTo[:, :, (i - 8) * 128:(i - 7) * 128]
        b_hT = [Buf("hT%d" % i) for i in range(16)]
        KT = cv(A2, 32 * KB, BF16).rearrange("p (h t) -> p h t", h=8)
        b_KT2 = [[Buf("KT%d_%d" % (h, g)) for g in range(4)] for h in range(8)]
        b_KT = [b for row in b_KT2 for b in row]
        Vv = cv(A3, 32 * KB, BF16).rearrange("p (i n) -> p i n", i=16)
        b_V2 = [[Buf("V%d_%d" % (i, g)) for g in range(2)] for i in range(16)]
        b_V = [b for row in b_V2 for b in row]

        g1B = cv(A2, 8 * KB)
        xs = [cv(A3 + i * 8 * KB, 8 * KB) for i in range(3)]
        xn = [cv(A3 + 24 * KB + i * 4 * KB, 4 * KB, BF16) for i in range(2)]
        junk = cv(TR + 16 * KB, 4 * KB, BF16)
        b_g1B, b_junk = Buf("g1B"), Buf("junk")
        b_xs = [Buf(), Buf(), Buf()]
        b_xn = [Buf(), Buf()]
        b_ssl = [Buf("ss%d" % i) for i in range(16)]
        S.dma("sp", lambda e: e.dma_start(out=g1B, in_=g1B_d), writes=[b_g1B], semkey="c_g1B")

        def rms_stats(src_ap, b_src, col, width):
            S.op("act", lambda e: e.activation(out=junk[:, 0:width], in_=src_ap, func=AF.Square, accum_out=ssv[:, col:col + 1]),
                 reads=[b_src], writes=[b_junk, b_ssl[col]])
            S.op("act", lambda e: e.activation(out=rtv[:, col:col + 1], in_=ssv[:, col:col + 1], func=AF.Sqrt, scale=1.0 / width, bias=epsc),
                 reads=[b_ssl[col], b_const], writes=[b_ssl[col]])
            S.op("dve", lambda e: e.reciprocal(rstdv[:, col:col + 1], rtv[:, col:col + 1]), reads=[b_ssl[col]], writes=[b_ssl[col]])

        def phase1_tile(i):
            sl = i % 2
            s3 = i % 3
            src = (xp if i < 8 else xo)[(i % 8) * 128:(i % 8 + 1) * 128, :]
            S.dma("sp", lambda e: e.dma_start(out=xs[s3], in_=src), writes=[b_xs[s3]], semkey=("xs", s3))
            rms_stats(xs[s3], b_xs[s3], i, 2048)
            S.op("dve", lambda e: e.scalar_tensor_tensor(out=xn[sl], in0=xs[s3], scalar=rstdv[:, i:i + 1], in1=g1B, op0=ALU.mult, op1=ALU.mult),
                 reads=[b_xs[s3], b_ssl[i], b_g1B], writes=[b_xn[sl]])
            pst = ps[:, 2 * sl:2 * sl + 2, :].bitcast(BF16).rearrange("p a (k n) -> p (a k) n", n=128)
            for k in range(16):
                S.op("pe", (lambda e, k=k: e.transpose(pst[:, k, :], xn[sl][:, k * 128:(k + 1) * 128], ident_bf)),
                     reads=[b_xn[sl], b_const], writes=[bps[2 * sl], bps[2 * sl + 1]])
            eng = "act" if i % 2 == 0 else "dve"
            if eng == "act":
                S.op("act", lambda e: e.activation(out=hTall(i), in_=pst, func=AF.Copy),
                     reads=[bps[2 * sl], bps[2 * sl + 1]], writes=[b_hT[i]])
            else:
                S.op("dve", lambda e: e.tensor_copy(hTall(i), pst),
                     reads=[bps[2 * sl], bps[2 * sl + 1]], writes=[b_hT[i]])

        for i in range(16):
            phase1_tile(i)

        dumps = []

        def dump(name, ap, bufs):
            if name in dbg_d:
                dumps.append((name, ap, bufs))

        alias(b_KT, [b_g1B])
        alias(b_V, b_xs + b_xn)
        tsq = [cv(TR + i * KB, KB, BF16) for i in range(2)]
        trt = [cv(TR + 2 * KB + i * 2 * KB, 2 * KB) for i in range(2)]
        b_tsq = [Buf(), Buf()]
        b_trt = [Buf(), Buf()]
        rot = {"a": 0, "b": 0, "u": 0}

        def qk_unit(wslot, b_w, hh, dstT, b_dst, h, t0, hoff, gcol):
            ba = rot["a"] % 4
            rot["a"] += 1
            bb = 4 + rot["b"] % 2
            rot["b"] += 1
            u = rot["u"] % 2
            rot["u"] += 1
            w3 = wslot.rearrange("p (k n) -> p k n", k=16)
            tiles = [b_hT[(hoff + t0) // 128 + j] for j in range(4)]
            for k in range(16):
                S.op("pe", (lambda e, k=k: e.matmul(ps[:, ba, :], w3[:, k, hh * 128:(hh + 1) * 128], hTs(k, hoff + t0, 512), start=(k == 0), stop=(k == 15))),
                     reads=[b_w] + tiles, writes=[bps[ba]])
            def norm():
                S.op("act", lambda e: e.activation(out=tsq[u], in_=ps[:, ba, :], func=AF.Square), reads=[bps[ba]], writes=[b_tsq[u]])
                S.op("pe", lambda e: e.matmul(ps[:, bb, :], ones_bf, tsq[u], start=True, stop=True), reads=[b_tsq[u], b_const], writes=[bps[bb]])
                S.op("act", lambda e: e.activation(out=trt[u], in_=ps[:, bb, :], func=AF.Ln, scale=1.0 / 128, bias=epsc), reads=[bps[bb], b_const], writes=[b_trt[u]])
                S.op("act", lambda e: e.activation(out=trt[u], in_=trt[u], func=AF.Exp, scale=-0.5), reads=[b_trt[u]], writes=[b_trt[u]])
                S.op("dve", lambda e: e.scalar_tensor_tensor(out=dstT[:, h, t0:t0 + 512], in0=ps[:, ba, :], scalar=gcol, in1=trt[u], op0=ALU.mult, op1=ALU.mult),
                     reads=[bps[ba], b_trt[u], b_const], writes=[b_dst])
            prev = qkp["pending"]
            qkp["pending"] = norm
            if prev is not None:
                prev()

        qkp = {"pending": None}

        def qk_flush():
            if qkp["pending"] is not None:
                qkp["pending"]()
                qkp["pending"] = None

        for blk in range(2):
            wslot, b_w = ws_take()
            for hh in range(4):
                for tg in range(4):
                    qk_unit(wslot, b_w, hh, KT, b_KT2[blk * 4 + hh][tg], blk * 4 + hh, tg * 512, 0, kg)
            ws_release()
        qk_flush()
        evi = [0]

        def evac(out_ap, in_ap, reads, writes, eng=None):
            evi[0] += 1
            if eng == "act" or (eng is None and evi[0] % 2 == 0):
                S.op("act", lambda e: e.activation(out=out_ap, in_=in_ap, func=AF.Copy), reads=reads, writes=writes)
            else:
                S.op("dve", lambda e: e.tensor_copy(out_ap, in_ap), reads=reads, writes=writes)

        def v_unit(wslot, b_w, blk, i):
            w3 = wslot.rearrange("p (k n) -> p k n", k=16)
            ba = rot["a"] % 4
            rot["a"] += 1
            for k in range(16):
                S.op("pe", (lambda e, k=k: e.matmul(ps[:, ba, :], hTs(k, i * 128, 128), w3[:, k, :], start=(k == 0), stop=(k == 15))),
                     reads=[b_w, b_hT[i]], writes=[bps[ba]])
            evac(Vv[:, i, blk * 512:(blk + 1) * 512], ps[:, ba, :], [bps[ba]], [b_V2[i][blk]])

        for blk in range(2):
            wslot, b_w = ws_take()
            for i in range(16):
                v_unit(wslot, b_w, blk, i)
            ws_release()
        b_f = Buf("f")
        for i in range(16):
            for k in range(16):
                S.op("pe", (lambda e, k=k, i=i: e.matmul(ps[:, 7, i * 8:(i + 1) * 8], hTs(k, i * 128, 128), wf[:, k, :], start=(k == 0), stop=(k == 15))),
                     reads=[b_const, b_hT[i]], writes=[bps[7]])
        S.op("dve", lambda e: e.tensor_tensor(out=fz, in0=ps[:, 7, 0:128], in1=bfB, op=ALU.add), reads=[bps[7], b_const], writes=[b_f])
        S.op("act", lambda e: e.activation(out=fz, in_=fz, func=AF.Exp, scale=-1.0), reads=[b_f], writes=[b_f])
        S.op("act", lambda e: e.activation(out=spt, in_=fz, func=AF.Ln, bias=onec), reads=[b_f, b_const], writes=[b_f])
        b_C = Buf("C")
        for i in range(16):
            S.op("pe", (lambda e, i=i: e.matmul(ps[:, 5, i * 8:(i + 1) * 8], ones_f, spt[:, i * 8:(i + 1) * 8], start=True, stop=True)),
                 reads=[b_f, b_const], writes=[bps[5]])
        for i in range(16):
            S.op("pe", (lambda e, i=i: e.matmul(ps[:, 6, i * 8:(i + 1) * 8], triU_f, spt[:, i * 8:(i + 1) * 8], start=True, stop=True)),
                 reads=[b_f, b_const], writes=[bps[6]])
        S.op("dve", lambda e: e.tensor_copy(fz, ps[:, 5, 0:128]), reads=[bps[5], b_f], writes=[b_f])
        S.op("dve", lambda e: e.memset(pre[:, 0:8], 0.0), writes=[b_C])
        for i in range(1, 16):
            S.op("dve", (lambda e, i=i: e.tensor_tensor(out=pre[:, i * 8:(i + 1) * 8], in0=pre[:, (i - 1) * 8:i * 8], in1=fz[:, (i - 1) * 8:i * 8], op=ALU.add)),
                 reads=[b_f, b_C], writes=[b_C])
        S.op("dve", lambda e: e.tensor_tensor(out=Cc, in0=ps[:, 6, 0:128], in1=pre, op=ALU.add), reads=[bps[6], b_C], writes=[b_C])
        b_bt = Buf("attbias")
        S.op("dve", lambda e: e.tensor_scalar(Cm[:, 0:64], Cc[:, 0:64], maskb, None, ALU.add), reads=[b_C, b_const], writes=[b_bt])
        S.op("dve", lambda e: e.tensor_copy(Cm[:, 64:128], Cc[:, 64:128]), reads=[b_C], writes=[b_bt])
        b_off = Buf("offT")
        alias([b_off], b_tsq + b_trt + [b_junk])
        S.op("pool", lambda e: e.iota(iotaH, [[1, 8], [0, 128]], base=0, channel_multiplier=0, allow_small_or_imprecise_dtypes=True), writes=[b_off])
        S.op("dve", lambda e: e.tensor_scalar(selAll.rearrange("p h j -> p (h j)"), iotaH, pidx, None, ALU.is_equal), reads=[b_off, b_iota], writes=[b_off])
        S.op("dve", lambda e: e.memset(offT, 0.0), writes=[b_off])
        psO = psf(4, 2)
        for i in range(8):
            S.op("pe", (lambda e, i=i: e.transpose(psO[0:8, i * 128:(i + 1) * 128], Cc[:, (8 + i) * 8:(9 + i) * 8], ident_f)),
                 reads=[b_C, b_const], writes=[bps[4 + i // 4]])
        S.op("dve", lambda e: e.tensor_scalar(offT[0:8, :], psO[0:8, :], -1.0 / SCALE, None, ALU.mult), reads=[bps[4], bps[5], b_off], writes=[b_off])
        dump("KT", KT, b_KT)
        dump("V", Vv, b_V)
        dump("Cc", Cc, [b_C])

        QT = cv(A0, 16 * KB, BF16).rearrange("p (h t) -> p h t", h=8)
        b_QT2 = [[Buf("QT%d_%d" % (h, g)) for g in range(2)] for h in range(8)]
        b_QT = [b for row in b_QT2 for b in row]
        alias(b_QT, b_hT[0:8])
        for blk in range(2):
            wslot, b_w = ws_take()
            for hh in range(4):
                for tg in range(2):
                    qk_unit(wslot, b_w, hh, QT, b_QT2[blk * 4 + hh][tg], blk * 4 + hh, tg * 512, 1024, qg)
            ws_release()
        qk_flush()
        if NCONV_E > 0:
            conv_burst()
        dump("QT", QT, b_QT)
        if upto <= 4:
            return finish(nc, S, dumps, dbg_d)


        oT = cv(A0 + 16 * KB, 16 * KB, BF16).rearrange("p (h t) -> p h t", h=8)
        b_oT = [Buf("oT%d" % h) for h in range(8)]
        alias(b_oT, b_hT[0:8])
        PT = [cv(TR + i * 2 * KB, 2 * KB, BF16) for i in range(3)]
        b_PT = [Buf() for _ in range(3)]
        rinv2 = [cv(TR + 6 * KB, 2 * KB), cv(TR + 20 * KB, 2 * KB)]
        b_rinv2 = [Buf(), Buf()]
        b_rinv = b_rinv2[0]
        alias(b_PT + b_rinv2, b_tsq + b_trt)

        PTq = [cv(TR + i * KB, KB, BF16) for i in range(6)]
        b_PT.extend([Buf(), Buf(), Buf()])
        b_PTq = b_PT
        alias(b_PTq[3:], b_tsq + b_trt)

        def step_geom(qh, kt):
            q0 = max(qh * 512, max(0, kt - 8) * 128)
            q1 = (qh + 1) * 512
            return q0, q1, q0 - qh * 512, q1 - qh * 512

        def att_S(h, qh, kt, sbank):
            q0, q1, l0, l1 = step_geom(qh, kt)
            dq = (kt - 8) * 128
            diag = (kt >= 8 and qh * 512 <= dq < (qh + 1) * 512)
            wr = [bps[sbank]]
            S.op("pe", lambda e: e.matmul(ps[:, sbank, l0:l1], KT[:, h, kt * 128:(kt + 1) * 128], QT[:, h, q0:q1], start=True, stop=False),
                 reads=[b_KT2[h][kt // 4], b_QT2[h][qh]], writes=wr)
            S.op("pe", lambda e: e.matmul(ps[:, sbank, l0:l1], selAll[:, h, :], offT[:, q0:q1], start=False, stop=(not diag)), reads=[b_off], writes=wr)
            if diag:
                S.op("pe", lambda e: e.matmul(ps[:, sbank, dq - qh * 512:dq - qh * 512 + 128], ident_bf, negmask_bf, start=False, stop=True), reads=[b_const], writes=wr)

        def att_P(h, qh, kt, sbank, slot):
            q0, q1, l0, l1 = step_geom(qh, kt)
            S.op("act", lambda e: e.activation(out=PTq[slot][:, l0:l1], in_=ps[:, sbank, l0:l1], func=AF.Exp, scale=SCALE, bias=Cm[:, kt * 8 + h:kt * 8 + h + 1]),
                 reads=[bps[sbank], b_bt], writes=[b_PTq[slot]])

        def att_V(h, qh, kt, slot):
            q0, q1, l0, l1 = step_geom(qh, kt)
            first = (kt == 0)
            last = (kt == (11 if qh == 0 else 15))
            S.op("pe", lambda e: e.matmul(ps[:, 4 + qh, l0:l1], Vv[:, kt, h * 128:(h + 1) * 128], PTq[slot][:, l0:l1], start=first, stop=last),
                 reads=[b_V2[kt][h // 4], b_PTq[slot]], writes=[bps[4 + qh]])
            S.op("pe", lambda e: e.matmul(ps[:, 6 + qh, l0:l1], ones_bf, PTq[slot][:, l0:l1], start=first, stop=last),
                 reads=[b_PTq[slot], b_const], writes=[bps[6 + qh]])

        def att_fin(h, qh):
            S.op("dve", lambda e: e.reciprocal(rinv2[qh], ps[:, 6 + qh, :]), reads=[bps[6 + qh]], writes=[b_rinv2[qh]])
            S.op("dve", lambda e: e.tensor_tensor(out=oT[:, h, qh * 512:(qh + 1) * 512], in0=ps[:, 4 + qh, :], in1=rinv2[qh], op=ALU.mult),
                 reads=[bps[4 + qh], b_rinv2[qh]], writes=[b_oT[h]])

        steps = [(h, qh, kt) for h in range(8) for qh in range(2) for kt in range(12 if qh == 0 else 16)]
        LOOK = 3
        for idx in range(min(LOOK, len(steps))):
            att_S(*steps[idx], idx % 4)
        for idx, (h, qh, kt) in enumerate(steps):
            if idx + LOOK < len(steps):
                att_S(*steps[idx + LOOK], (idx + LOOK) % 4)
            att_P(h, qh, kt, idx % 4, idx % 6)
            att_V(h, qh, kt, idx % 6)
            if kt == (11 if qh == 0 else 15):
                att_fin(h, qh)
        dump("oT", oT, b_oT)
        if upto <= 5:
            return finish(nc, S, dumps, dbg_d)

        uT = cv(A2, 16 * KB, BF16).rearrange("p (g t) -> p g t", g=8)
        vn = cv(A2 + 16 * KB, 16 * KB, BF16).rearrange("p (i n) -> p i n", i=8)
        b_uT = [Buf("uT%d" % g) for g in range(8)]
        b_vn = [Buf("vn%d" % i) for i in range(8)]
        alias(b_uT + b_vn, b_KT)
        lngB = cv(A3, 4 * KB)
        lnbB = cv(A3 + 4 * KB, 4 * KB)
        bspB = cv(A3 + 8 * KB, 4 * KB)
        wsTf = cv(A3 + 12 * KB, 4 * KB)
        wsTm = cv(A3 + 16 * KB, 2 * KB, BF16).rearrange("p (g t) -> p g t", g=8)
        vg = [cv(A3 + 18 * KB + i * 4 * KB, 4 * KB) for i in range(2)]
        tmpS = cv(A3 + 26 * KB, 4 * KB)
        b_sguc, b_wsm, b_tmpS = Buf("sguc"), Buf("wsm"), Buf("tmpS")
        b_vg = [Buf(), Buf()]
        alias([b_sguc, b_wsm, b_tmpS] + b_vg, b_V)
        for dst, src, key in ((lngB, lngB_d, "lng"), (lnbB, lnbB_d, "lnb"), (bspB, bspB_d, "bsp"), (wsTf, wsT_d, "wst")):
            S.dma("sp", (lambda e, dst=dst, src=src: e.dma_start(out=dst, in_=src)), writes=[b_sguc], semkey="c_sgu", chain=(key != "lng"))
        for g in range(8):
            S.op("dve", (lambda e, g=g: e.tensor_tensor(out=wsTm[:, g, :], in0=wsTf[:, g * 128:(g + 1) * 128], in1=triU_f, op=ALU.mult)),
                 reads=[b_sguc, b_const], writes=[b_wsm])
        bst = cv(TR + 8 * KB, 96)
        mv = cv(TR + 8 * KB + 96, 32)
        b_st = Buf("st")

        def u_unit(wslot, b_w, gg, g, tg):
            w3 = wslot.rearrange("p (k n) -> p k n", k=16)
            ba = rot["a"] % 4
            rot["a"] += 1
            for k in range(16):
                S.op("pe", (lambda e, k=k: e.matmul(ps[:, ba, :], w3[:, k, gg * 128:(gg + 1) * 128], hTo[:, k, tg * 512:(tg + 1) * 512], start=(k == 0), stop=(k == 15))),
                     reads=[b_w] + b_hT[8 + tg * 4:12 + tg * 4], writes=[bps[ba]])
            S.op("act", lambda e: e.activation(out=uT[:, g, tg * 512:(tg + 1) * 512], in_=ps[:, ba, :], func=AF.Gelu_apprx_tanh), reads=[bps[ba]], writes=[b_uT[g]])

        for blk in range(2):
            wslot, b_w = ws_take()
            for gg in range(4):
                for tg in range(2):
                    u_unit(wslot, b_w, gg, blk * 4 + gg, tg)
            ws_release()

        def v_proj_ln(w0, b_w0, w1, b_w1, i):
            sl = i % 2
            for blk, (wslot, b_w) in enumerate(((w0, b_w0), (w1, b_w1))):
                w3 = wslot.rearrange("p (k n) -> p k n", k=16)
                ba = 4 + 2 * sl + blk
                for k in range(16):
                    S.op("pe", (lambda e, k=k, w3=w3, ba=ba: e.matmul(ps[:, ba, :], hTo[:, k, i * 128:(i + 1) * 128], w3[:, k, :], start=(k == 0), stop=(k == 15))),
                         reads=[b_w, b_hT[8 + i]], writes=[bps[ba]])
            S.op("act", lambda e: e.activation(out=vg[sl], in_=psf(4 + 2 * sl, 2), func=AF.Gelu_apprx_tanh), reads=[bps[4 + 2 * sl], bps[5 + 2 * sl]], writes=[b_vg[sl]])
            bs_, mv_ = bst[:, sl * 12:sl * 12 + 12], mv[:, sl * 4:sl * 4 + 4]
            S.op("dve", lambda e: e.bn_stats(bs_[:, 0:6], vg[sl][:, 0:512]), reads=[b_vg[sl]], writes=[b_st2[sl]])
            S.op("dve", lambda e: e.bn_stats(bs_[:, 6:12], vg[sl][:, 512:1024]), reads=[b_vg[sl]], writes=[b_st2[sl]])
            S.op("dve", lambda e: e.bn_aggr(mv_[:, 0:2], bs_.rearrange("p (a b) -> p a b", b=6)), reads=[b_st2[sl]], writes=[b_st2[sl]])
            S.op("act", lambda e: e.activation(out=mv_[:, 2:3], in_=mv_[:, 1:2], func=AF.Sqrt, scale=1.0, bias=epsc), reads=[b_st2[sl], b_const], writes=[b_st2[sl]])
            S.op("dve", lambda e: e.reciprocal(mv_[:, 3:4], mv_[:, 2:3]), reads=[b_st2[sl]], writes=[b_st2[sl]])
            S.op("dve", lambda e: e.tensor_scalar(vg[sl], vg[sl], mv_[:, 0:1], mv_[:, 3:4], ALU.subtract, ALU.mult), reads=[b_st2[sl], b_vg[sl]], writes=[b_vg[sl]])
            S.op("dve", lambda e: e.tensor_tensor(out=vg[sl], in0=vg[sl], in1=lngB, op=ALU.mult), reads=[b_vg[sl], b_sguc], writes=[b_vg[sl]])
            S.op("dve", lambda e: e.tensor_tensor(out=vn[:, i, :], in0=vg[sl], in1=lnbB, op=ALU.add), reads=[b_vg[sl], b_sguc], writes=[b_vn[i]])

        def v_spatial(i):
            sl = i % 2
            psX = psf(0 + 2 * sl, 2)
            for g in range(8):
                S.op("pe", (lambda e, g=g: e.matmul(psX[:, g * 128:(g + 1) * 128], vn[:, i, g * 128:(g + 1) * 128], wsTm[:, g, :], start=True, stop=True)),
                     reads=[b_vn[i], b_wsm], writes=[bps[2 * sl + g // 4]])
            S.op("dve", lambda e: e.tensor_tensor(out=tmpS, in0=psX, in1=bspB, op=ALU.add), reads=[bps[2 * sl], bps[2 * sl + 1], b_sguc], writes=[b_tmpS])
            S.op("dve", lambda e: e.tensor_tensor(out=uT[:, :, i * 128:(i + 1) * 128], in0=tmpS.rearrange("p (g t) -> p g t", g=8),
                                                  in1=uT[:, :, i * 128:(i + 1) * 128], op=ALU.mult),
                 reads=[b_tmpS] + b_uT, writes=b_uT)

        b_st2 = [b_st, Buf("st2")]
        w0, b_w0 = ws_take()
        w1, b_w1 = ws_take()
        v_proj_ln(w0, b_w0, w1, b_w1, 0)
        for i in range(8):
            if i + 1 < 8:
                v_proj_ln(w0, b_w0, w1, b_w1, i + 1)
            v_spatial(i)
        ws_release()
        ws_release()
        dump("suT", uT, b_uT)
        if upto <= 6:
            return finish(nc, S, dumps, dbg_d)

        mT = cv(A3, 32 * KB, BF16).rearrange("p (c t) -> p c t", c=16)
        b_mT2 = [[Buf("mT%d_%d" % (c_, g)) for g in range(2)] for c_ in range(16)]
        b_mT = [b for row in b_mT2 for b in row]
        alias(b_mT, [b_sguc, b_wsm, b_tmpS] + b_vg)
        sgS = [cv(TR, 8 * KB, BF16).rearrange("p (c t) -> p c t", c=4), cv(A2 + 16 * KB, 8 * KB, BF16).rearrange("p (c t) -> p c t", c=4)]
        sgF = [cv(TR + 8 * KB, 8 * KB, BF16).rearrange("p (c t) -> p c t", c=4), cv(A2 + 24 * KB, 8 * KB, BF16).rearrange("p (c t) -> p c t", c=4)]
        m1t = [cv(A0 + i * 4 * KB, 2 * KB) for i in range(2)]
        m2t = [cv(A0 + i * 4 * KB + 2 * KB, 2 * KB) for i in range(2)]
        b_sgS3 = [[[Buf() for g in range(2)] for cc in range(4)] for p_ in range(2)]
        b_sgF3 = [[[Buf() for g in range(2)] for cc in range(4)] for p_ in range(2)]
        b_sgS = [[b for r in b_sgS3[p_] for b in r] for p_ in range(2)]
        b_sgF = [[b for r in b_sgF3[p_] for b in r] for p_ in range(2)]
        b_g7 = [Buf(), Buf()]
        alias(b_sgS[0] + b_sgS[1] + b_sgF[0] + b_sgF[1] + b_g7, b_PT + b_rinv2 + [b_st, b_st2[1], b_bt, b_off] + b_vn + b_QT)
        g7 = {"u": 0}

        def sig_unit(wg, b_wg, dst, b_dst, cc, bcol, tg):
            ba = rot["a"] % 8
            rot["a"] += 1
            wg3 = wg.rearrange("p (k n) -> p k n", k=16)
            tsl = slice(tg * 512, (tg + 1) * 512)
            csl = slice(cc * 128, (cc + 1) * 128)
            for k in range(16):
                S.op("pe", (lambda e, k=k: e.matmul(ps[:, ba, :], wg3[:, k, csl], hTo[:, k, tsl], start=(k == 0), stop=(k == 15))),
                     reads=[b_wg] + b_hT[8 + tg * 4:12 + tg * 4], writes=[bps[ba]])
            S.op("act", lambda e: e.activation(out=dst[:, cc, tsl], in_=ps[:, ba, :], func=AF.Sigmoid, bias=bgT[:, bcol:bcol + 1]), reads=[bps[ba], b_const], writes=[b_dst])

        def proj_unit(wpp, b_wpp, sS, b_sS, sF, b_sF, cc, c_, tg):
            u = g7["u"] % 2
            g7["u"] += 1
            b0 = rot["a"] % 8
            rot["a"] += 1
            b1 = rot["a"] % 8
            rot["a"] += 1
            wpp3 = wpp.rearrange("p (k n) -> p k n", k=16)
            tsl = slice(tg * 512, (tg + 1) * 512)
            csl = slice(cc * 128, (cc + 1) * 128)
            for k in range(8):
                S.op("pe", (lambda e, k=k: e.matmul(ps[:, b0, :], wpp3[:, k, csl], uT[:, k, tsl], start=(k == 0), stop=(k == 7))),
                     reads=[b_wpp, b_uT[k]], writes=[bps[b0]])
            for k in range(8):
                S.op("pe", (lambda e, k=k: e.matmul(ps[:, b1, :], wpp3[:, 8 + k, csl], oT[:, k, tsl], start=(k == 0), stop=(k == 7))),
                     reads=[b_wpp, b_oT[k]], writes=[bps[b1]])
            S.op("dve", lambda e: e.tensor_tensor(out=m1t[u], in0=sS[:, cc, tsl], in1=ps[:, b0, :], op=ALU.mult), reads=[b_sS, bps[b0]], writes=[b_g7[u]])
            S.op("dve", lambda e: e.tensor_tensor(out=m2t[u], in0=sF[:, cc, tsl], in1=ps[:, b1, :], op=ALU.mult), reads=[b_sF, bps[b1]], writes=[b_g7[u]])
            S.op("dve", lambda e: e.tensor_tensor(out=mT[:, c_, tsl], in0=m1t[u], in1=m2t[u], op=ALU.add), reads=[b_g7[u]], writes=[b_mT2[c_][tg]])

        for n in range(4):
            p = n % 2
            wgs, b_wgs = ws_take()
            for cc in range(4):
                for tg in range(2):
                    sig_unit(wgs, b_wgs, sgS[p], b_sgS3[p][cc][tg], cc, n * 4 + cc, tg)
            ws_release()
            wgf, b_wgf = ws_take()
            for cc in range(4):
                for tg in range(2):
                    sig_unit(wgf, b_wgf, sgF[p], b_sgF3[p][cc][tg], cc, 16 + n * 4 + cc, tg)
            ws_release()
            wpp, b_wpp = ws_take()
            for cc in range(4):
                for tg in range(2):
                    proj_unit(wpp, b_wpp, sgS[p], b_sgS3[p][cc][tg], sgF[p], b_sgF3[p][cc][tg], cc, n * 4 + cc, tg)
            ws_release()
        dump("mT", mT, b_mT)
        if upto <= 7:
            return finish(nc, S, dumps, dbg_d)

        x1 = cv(A0, 64 * KB).rearrange("p (i n) -> p i n", i=8)
        b_x1 = [Buf("x1_%d" % i) for i in range(8)]
        alias(b_x1, b_QT + b_oT + b_hT[8:16] + b_g7)
        for i in range(8):
            S.dma("sp", (lambda e, i=i: e.dma_start(out=x1[:, i, :], in_=xo[i * 128:(i + 1) * 128, :])), writes=[b_x1[i]], semkey=("x1", i))

        def out_unit(wo, b_wo, n, i):
            wo3 = wo.rearrange("p (k n) -> p k n", k=16)
            ba = rot["a"] % 8
            rot["a"] += 1
            for c_ in range(16):
                S.op("pe", (lambda e, c_=c_: e.matmul(ps[:, ba, :], mT[:, c_, i * 128:(i + 1) * 128], wo3[:, c_, :], start=(c_ == 0), stop=(c_ == 15))),
                     reads=[b_wo, b_mT2[c_][i // 4]], writes=[bps[ba]])
            S.op("dve", lambda e: e.tensor_tensor(out=x1[:, i, n * 512:(n + 1) * 512], in0=x1[:, i, n * 512:(n + 1) * 512], in1=ps[:, ba, :], op=ALU.add),
                 reads=[bps[ba], b_x1[i]], writes=[b_x1[i]])

        for n in range(4):
            wo, b_wo = ws_take()
            for i in range(8):
                out_unit(wo, b_wo, n, i)
            ws_release()
        dump("x1", x1, b_x1)
        if upto <= 8:
            return finish(nc, S, dumps, dbg_d)

        h2 = cv(A2, 32 * KB, BF16).rearrange("p (i n) -> p i n", i=8)
        b_h2 = [Buf("h2_%d" % i) for i in range(8)]
        alias(b_h2, b_uT + b_vn + b_sgS[1] + b_sgF[1])
        g2B = cv(A3, 8 * KB)
        wr3 = cv(A3 + 8 * KB, 2304).rearrange("p (k n) -> p k n", k=16)
        brB = cv(A3 + 8 * KB + 2304, 144)
        h2f_l = [cv(A3 + 12 * KB, 8 * KB), cv(TR + 6400, 8 * KB)]
        h2Tf_l = [cv(A3 + 20 * KB, 8 * KB), cv(TR + 6400 + 8 * KB, 8 * KB)]
        junk2 = cv(A3 + 28 * KB, 4 * KB, BF16)
        b_p9c, b_h2f, b_h2Tf, b_junk2 = Buf("p9c"), Buf("h2f"), Buf("h2Tf"), Buf("junk2")
        alias([b_p9c, b_h2f, b_h2Tf, b_junk2], b_mT)
        b_h2f_l = [b_h2f, Buf("h2f1")]
        b_h2Tf_l = [[b_h2Tf, Buf("h2Tf0b")], [Buf("h2Tf1a"), Buf("h2Tf1b")]]
        alias([b_h2f_l[1]] + b_h2Tf_l[1], [b_bt, b_off, b_st, b_st2[1], b_g1B] + b_g7 + b_sgS[0] + b_sgS[1] + b_sgF[0] + b_sgF[1] + b_PT + b_rinv2 + b_tsq + b_trt)
        alias([b_h2Tf_l[0][1]], b_mT)
        S.dma("sp", lambda e: e.dma_start(out=g2B, in_=g2B_d), writes=[b_p9c], semkey="c_g2B")
        S.dma("sp", lambda e: e.dma_start(out=wr3, in_=wr_d.rearrange("(k p) n -> p k n", p=128)), writes=[b_p9c], semkey="c_wr")
        S.dma("sp", lambda e: e.dma_start(out=brB, in_=brB_d), writes=[b_p9c], semkey="c_brB")
        Lg = cv(TR, 1152).rearrange("p (i n) -> p i n", i=8)
        posm = cv(TR + 1280, 1024).rearrange("p (i n) -> p i n", i=8)
        gate = cv(TR + 2304, 1024).rearrange("p (i n) -> p i n", i=8)
        Mm = cv(TR + 3328, 1024).rearrange("p (i n) -> p i n", i=8)
        scr = [cv(TR + 4352 + i * 1024, 1024) for i in range(2)]
        b_L, b_tab, b_M = Buf("L"), Buf("tab"), Buf("M")
        b_scr = [Buf(), Buf()]
        alias([b_L, b_tab, b_M] + b_scr, [b_h2f_l[1]] + b_h2Tf_l[1] + b_g7 + b_sgS[0] + b_sgS[1] + b_sgF[0] + b_sgF[1])

        def p9_tile(i):
            S.op("act", lambda e: e.activation(out=junk2, in_=x1[:, i, :], func=AF.Square, accum_out=ssv[:, i:i + 1]), reads=[b_x1[i]], writes=[b_junk2, b_ssl[i]])
            S.op("act", lambda e: e.activation(out=rtv[:, i:i + 1], in_=ssv[:, i:i + 1], func=AF.Sqrt, scale=1.0 / 2048, bias=epsc), reads=[b_ssl[i], b_const], writes=[b_ssl[i]])
            S.op("dve", lambda e: e.reciprocal(rstdv[:, i:i + 1], rtv[:, i:i + 1]), reads=[b_ssl[i]], writes=[b_ssl[i]])
            h2f, h2Tf = h2f_l[i % 2], h2Tf_l[i % 2]
            bh2f, bh2Tf = b_h2f_l[i % 2], b_h2Tf_l[i % 2]
            S.op("dve", lambda e: e.scalar_tensor_tensor(out=h2f, in0=x1[:, i, :], scalar=rstdv[:, i:i + 1], in1=g2B, op0=ALU.mult, op1=ALU.mult),
                 reads=[b_x1[i], b_ssl[i], b_p9c], writes=[bh2f])
            S.op("act", lambda e: e.activation(out=h2[:, i, :], in_=h2f, func=AF.Copy), reads=[bh2f], writes=[b_h2[i]])
            psT = psf(0, 4)
            for k in range(16):
                S.op("pe", (lambda e, k=k: e.transpose(psT[:, k * 128:(k + 1) * 128], h2f[:, k * 128:(k + 1) * 128], ident_f)),
                     reads=[bh2f, b_const], writes=[bps[k // 4]])
            S.op("act", lambda e: e.activation(out=h2Tf[:, 0:1024], in_=psT[:, 0:1024], func=AF.Copy), reads=[bps[0], bps[1]], writes=[bh2Tf[0]])
            S.op("dve", lambda e: e.tensor_copy(h2Tf[:, 1024:2048], psT[:, 1024:2048]), reads=[bps[2], bps[3]], writes=[bh2Tf[1]])
            bl = 4 + i % 2
            h2T3 = h2Tf.rearrange("p (k t) -> p k t", k=16)
            for k in range(16):
                S.op("pe", (lambda e, k=k: e.matmul(ps[:, bl, 0:36], h2T3[:, k, :], wr3[:, k, :], start=(k == 0), stop=(k == 15))),
                     reads=[bh2Tf[k // 8], b_p9c], writes=[bps[bl]])
            S.op("dve", lambda e: e.tensor_tensor(out=Lg[:, i, :], in0=ps[:, bl, 0:36], in1=brB, op=ALU.add), reads=[bps[bl], b_p9c], writes=[b_L])

        for i in range(8):
            p9_tile(i)

        alias(b_scr, [b_h2f_l[1]] + b_h2Tf_l[1])
        rs = TR + 6400
        def rsc(nfl):
            nonlocal_rs[0] += nfl * 4
            return cv(nonlocal_rs[0] - nfl * 4, nfl * 4)
        nonlocal_rs = [rs]
        r_gmax, r_gsum, r_pg, r_m1, r_m2, r_d, r_ed, r_den, r_w1, r_g1, r_g2 = [rsc(8) for _ in range(11)]
        r_goh, r_gsh, r_pen = [rsc(32).rearrange("p (i g) -> p i g", i=8) for _ in range(3)]
        r_em, r_oh1, r_em2, r_oh2 = [rsc(256).rearrange("p (i n) -> p i n", i=8) for _ in range(4)]
        assert nonlocal_rs[0] <= TR + 12 * KB
        bs = b_scr[0]
        gl3 = Lg[:, :, 0:4]
        el4 = Lg[:, :, 4:36].rearrange("p i (g e) -> p i g e", g=4)

        def bc8(ap, n):
            return ap.unsqueeze(2).broadcast_to([128, 8, n])

        def D(fn, reads=(), writes=()):
            S.op("dve", fn, reads=[bs] + list(reads), writes=[bs] + list(writes))

        fl = lambda ap: ap.rearrange("p i n -> p (i n)")
        D(lambda e: e.tensor_reduce(out=r_gmax, in_=gl3, axis=AX.X, op=ALU.max), reads=[b_L])
        D(lambda e: e.tensor_tensor(out=r_goh, in0=gl3, in1=bc8(r_gmax, 4), op=ALU.is_ge), reads=[b_L])
        D(lambda e: e.tensor_tensor(out=r_gsh, in0=gl3, in1=bc8(r_gmax, 4), op=ALU.subtract), reads=[b_L])
        S.op("act", lambda e: e.activation(out=fl(r_gsh), in_=fl(r_gsh), func=AF.Exp), reads=[bs], writes=[bs])
        D(lambda e: e.tensor_reduce(out=r_gsum, in_=r_gsh, axis=AX.X, op=ALU.add))
        D(lambda e: e.reciprocal(r_pg, r_gsum))
        D(lambda e: e.tensor_scalar(fl(r_pen), fl(r_goh), 1.0, BIG, ALU.subtract, ALU.mult))
        D(lambda e: e.tensor_tensor(out=r_em.rearrange("p i (g e) -> p i g e", g=4), in0=el4, in1=r_pen.unsqueeze(3).broadcast_to([128, 8, 4, 8]), op=ALU.add), reads=[b_L])
        D(lambda e: e.tensor_reduce(out=r_m1, in_=r_em, axis=AX.X, op=ALU.max))
        D(lambda e: e.tensor_tensor(out=r_oh1, in0=r_em, in1=bc8(r_m1, 32), op=ALU.is_ge))
        D(lambda e: e.scalar_tensor_tensor(out=fl(r_em2), in0=fl(r_oh1), scalar=-BIG, in1=fl(r_em), op0=ALU.mult, op1=ALU.add))
        D(lambda e: e.tensor_reduce(out=r_m2, in_=r_em2, axis=AX.X, op=ALU.max))
        D(lambda e: e.tensor_tensor(out=r_oh2, in0=r_em2, in1=bc8(r_m2, 32), op=ALU.is_ge))
        D(lambda e: e.tensor_tensor(out=r_d, in0=r_m2, in1=r_m1, op=ALU.subtract))
        S.op("act", lambda e: e.activation(out=r_ed, in_=r_d, func=AF.Exp), reads=[bs], writes=[bs])
        D(lambda e: e.tensor_scalar(r_den, r_ed, 1.0, None, ALU.add))
        D(lambda e: e.reciprocal(r_w1, r_den))
        D(lambda e: e.tensor_tensor(out=r_g1, in0=r_w1, in1=r_pg, op=ALU.mult))
        D(lambda e: e.tensor_tensor(out=r_g2, in0=r_g1, in1=r_ed, op=ALU.mult))
        D(lambda e: e.tensor_tensor(out=gate, in0=r_oh1, in1=bc8(r_g1, 32), op=ALU.mult), writes=[b_tab])
        D(lambda e: e.tensor_tensor(out=r_em, in0=r_oh2, in1=bc8(r_g2, 32), op=ALU.mult))
        D(lambda e: e.tensor_tensor(out=fl(gate), in0=fl(gate), in1=fl(r_em), op=ALU.add), reads=[b_tab], writes=[b_tab])
        D(lambda e: e.tensor_tensor(out=fl(Mm), in0=fl(r_oh1), in1=fl(r_oh2), op=ALU.add), writes=[b_M])
        for i in range(8):
            for i2 in range(i + 1):
                S.op("pe", (lambda e, i=i, i2=i2: e.matmul(ps[:, 6, i * 32:(i + 1) * 32], (triS_f if i2 == i else ones_f), Mm[:, i2, :], start=(i2 == 0), stop=(i2 == i))),
                     reads=[b_M, b_const], writes=[bps[6]])
        S.op("dve", lambda e: e.tensor_tensor(out=posm.rearrange("p i n -> p (i n)"), in0=ps[:, 6, 0:256], in1=Mm.rearrange("p i n -> p (i n)"), op=ALU.mult),
             reads=[bps[6], b_M], writes=[b_tab])
        S.op("dve", lambda e: e.scalar_tensor_tensor(out=posm.rearrange("p i n -> p (i n)"), in0=Mm.rearrange("p i n -> p (i n)"), scalar=-1.0,
                                                     in1=posm.rearrange("p i n -> p (i n)"), op0=ALU.add, op1=ALU.add),
             reads=[b_M, b_tab], writes=[b_tab])
        dump("L", Lg, [b_L])
        dump("posm", posm, [b_tab])
        dump("gate", gate, [b_tab])
        if upto <= 10:
            return finish(nc, S, dumps, dbg_d)

        Yb = [cv(A3 + q * 4 * KB, 4 * KB, BF16) for q in range(4)]
        SgT = [cv(A3 + 16 * KB + q * 2 * KB, 2 * KB, BF16).rearrange("p (j t) -> p j t", j=8) for q in range(8)]
        b_Yb2 = [[Buf(), Buf()] for _ in range(4)]
        b_Yb = [b for r in b_Yb2 for b in r]
        b_SgT = [Buf() for _ in range(8)]
        alias(b_Yb + b_SgT, [b_p9c, b_h2f, b_h2Tf, b_junk2, b_h2Tf_l[0][1]])
        sil = cv(TR + 4352, 2 * KB)
        T2 = TR + 6400
        Sm = cv(T2, 2 * KB, BF16).rearrange("p (j s) -> p j s", j=8)
        Sg = cv(T2 + 2 * KB, 2 * KB, BF16).rearrange("p (j s) -> p j s", j=8)
        AT = [cv(T2 + 4 * KB + i * KB, KB, BF16).rearrange("p (f s) -> p f s", f=4) for i in range(2)]
        Aa = [cv(T2 + 6 * KB + i * KB, KB, BF16) for i in range(2)]
        XeT = [cv(T2 + 8 * KB + i * 4 * KB, 4 * KB, BF16).rearrange("p (c s) -> p c s", c=16) for i in range(2)]
        assert T2 + 16 * KB <= 207 * KB
        b_Sm = [Buf("Sm%d" % j) for j in range(8)]
        b_Sg = [Buf("Sg%d" % j) for j in range(8)]
        b_sil = Buf("sil")
        b_AT = [Buf(), Buf()]
        b_Aa = [Buf(), Buf()]
        b_XeT2 = [[Buf(), Buf()] for _ in range(2)]
        b_XeT = [b for r in b_XeT2 for b in r]
        alias(b_Sm + b_Sg + [b_sil] + b_AT + b_Aa + b_XeT, [b_bt, b_off, b_st, b_st2[1], b_h2f_l[1]] + b_h2Tf_l[1] + b_g7 + b_sgS[0] + b_sgS[1] + b_sgF[0] + b_sgF[1] + b_PT + b_rinv2 + b_scr)
        urot = {"i": 0}

        def unit2():
            u = urot["i"] % 2
            urot["i"] += 1
            return 2 * u

        def onehots(ex):
            for j in range(8):
                S.op("dve", (lambda e, j=j: e.tensor_scalar(Sm[:, j, :], iota_f, posm[:, j, ex:ex + 1], None, ALU.is_equal)),
                     reads=[b_tab, b_const], writes=[b_Sm[j]])
            for j in range(8):
                S.op("dve", (lambda e, j=j: e.tensor_scalar(Sg[:, j, :], iota_f, posm[:, j, ex:ex + 1], gate[:, j, ex:ex + 1], ALU.is_equal, ALU.mult)),
                     reads=[b_tab, b_const], writes=[b_Sg[j]])

        def sgt_tr(ex):
            qq = ex % 8
            psTr = ps[:, 6:7, :].bitcast(BF16).rearrange("p a (j t) -> p (a j) t", t=128)
            for j in range(8):
                S.op("pe", (lambda e, j=j: e.transpose(psTr[:, j, :], Sg[:, j, :], ident_bf)), reads=[b_Sg[j], b_const], writes=[bps[6]])
            evac(SgT[qq], psTr, [bps[6]], [b_SgT[qq]], eng="act")

        def gather_half(ex, hf):
            sl = ex % 2
            b0 = unit2()
            psG = psf(b0, 2)
            for cc in range(8):
                c_ = hf * 8 + cc
                for j in range(8):
                    S.op("pe", (lambda e, cc=cc, c_=c_, j=j: e.matmul(psG[:, cc * 128:(cc + 1) * 128], h2[:, j, c_ * 128:(c_ + 1) * 128], Sm[:, j, :],
                                                                      start=(j == 0), stop=(j == 7))),
                         reads=[b_h2[j], b_Sm[j]], writes=[bps[b0 + cc // 4]])
            evac(XeT[sl][:, hf * 8:(hf + 1) * 8, :], psG.rearrange("p (c s) -> p c s", c=8), [bps[b0], bps[b0 + 1]], [b_XeT2[sl][hf]], eng="act")

        def gateup(ex):
            sl = ex % 2
            for bank in (4, 5):
                for half in range(2):
                    w, b_w = ws_take()
                    w3 = w.rearrange("p (k n) -> p k n", k=8)
                    for k in range(8):
                        c_ = half * 8 + k
                        S.op("pe", (lambda e, k=k, c_=c_, w3=w3, bank=bank: e.matmul(ps[:, bank, :], XeT[sl][:, c_, :], w3[:, k, :], start=(c_ == 0), stop=(c_ == 15))),
                             reads=[b_w, b_XeT2[sl][half]], writes=[bps[bank]])
                    ws_release()
            S.op("act", lambda e: e.activation(out=sil, in_=ps[:, 4, :], func=AF.Silu), reads=[bps[4]], writes=[b_sil])
            S.op("dve", lambda e: e.tensor_tensor(out=Aa[sl], in0=sil, in1=ps[:, 5, :], op=ALU.mult), reads=[b_sil, bps[5]], writes=[b_Aa[sl]])

        def a_tr(ex):
            sl = ex % 2
            psA = ps[:, 7:8, 0:256].bitcast(BF16).rearrange("p a (f s) -> p (a f) s", s=128)
            for fc in range(4):
                S.op("pe", (lambda e, fc=fc: e.transpose(psA[:, fc, :], Aa[sl][:, fc * 128:(fc + 1) * 128], ident_bf)), reads=[b_Aa[sl], b_const], writes=[bps[7]])
            evac(AT[sl], psA, [bps[7]], [b_AT[sl]], eng="act")

        def down(ex):
            sl = ex % 2
            q = ex % 4
            wlo, b_wlo = ws_take()
            whi, b_whi = ws_take()
            wl3 = wlo.rearrange("p (k n) -> p k n", k=2)
            wh3 = whi.rearrange("p (k n) -> p k n", k=2)
            for hf in range(2):
                b0 = unit2()
                for fc in range(4):
                    wsrc, bw = (wl3, b_wlo) if fc < 2 else (wh3, b_whi)
                    for nn in range(2):
                        n = hf * 2 + nn
                        S.op("pe", (lambda e, nn=nn, n=n, fc=fc, b0=b0, wsrc=wsrc: e.matmul(ps[:, b0 + nn, :], AT[sl][:, fc, :], wsrc[:, fc % 2, n * 512:(n + 1) * 512],
                                                                                            start=(fc == 0), stop=(fc == 3))),
                             reads=[bw, b_AT[sl]], writes=[bps[b0 + nn]])
                evac(Yb[q][:, hf * 1024:(hf + 1) * 1024], psf(b0, 2), [bps[b0], bps[b0 + 1]], [b_Yb2[q][hf]], eng="act")
            ws_release()
            ws_release()

        def combine(quad):
            for j in range(8):
                for hf in range(2):
                    b0 = unit2()
                    for nn in range(2):
                        n = hf * 2 + nn
                        for q in range(4):
                            qq = (quad % 2) * 4 + q
                            S.op("pe", (lambda e, nn=nn, n=n, q=q, qq=qq, b0=b0, j=j: e.matmul(ps[:, b0 + nn, :], SgT[qq][:, j, :], Yb[q][:, n * 512:(n + 1) * 512], start=(q == 0), stop=(q == 3))),
                                 reads=[b_SgT[qq], b_Yb2[q][hf]], writes=[bps[b0 + nn]])
                    S.op("dve", (lambda e, hf=hf, b0=b0, j=j: e.tensor_tensor(out=x1[:, j, hf * 1024:(hf + 1) * 1024], in0=x1[:, j, hf * 1024:(hf + 1) * 1024], in1=psf(b0, 2), op=ALU.add)),
                         reads=[bps[b0], bps[b0 + 1], b_x1[j]], writes=[b_x1[j]])

        NEXP = 32 if upto > 11 else 4
        onehots(0)
        sgt_tr(0)
        gather_half(0, 0)
        gather_half(0, 1)
        for s_ in range(NEXP):
            if s_ + 1 < NEXP:
                onehots(s_ + 1)
            gateup(s_)
            if s_ + 1 < NEXP:
                sgt_tr(s_ + 1)
                gather_half(s_ + 1, 0)
            a_tr(s_)
            if s_ + 1 < NEXP:
                gather_half(s_ + 1, 1)
            down(s_)
            if s_ % 4 == 3:
                combine(s_ // 4)
        dump("x1f", x1, b_x1)

        b_out = [Buf("out%d" % i) for i in range(8)]
        for i in range(8):
            S.dma("sp", (lambda e, i=i: e.dma_start(out=out_d[i * 128:(i + 1) * 128, :], in_=x1[:, i, :])), reads=[b_x1[i]], writes=[b_out[i]], semkey=("out", i))
        S.final_wait("sp", b_out)
        return finish(nc, S, dumps, dbg_d)
    return nc


def finish(nc, S, dumps, dbg_d):
    allb = []
    for name, ap, bufs in dumps:
        S.dma("pool", (lambda e, name=name, ap=ap: e.dma_start(out=dbg_d[name], in_=ap)), reads=bufs, writes=bufs, semkey="dump_" + name)
        allb.extend(bufs)
    S.final_wait("pool", allb)
    S.run()
    return nc


def prep_inputs(x, norm1_g, w_in, b_gate, b_forget, sgu_ln_g, sgu_ln_b, w_spatial, b_spatial,
                q_norm_g, k_norm_g, w_proj_sgu, w_proj_fox, w_out, norm2_g,
                w_router_group, b_router_group, w_router_expert, b_router_expert,
                w_expert_gate, w_expert_up, w_expert_down):
    f = lambda a: np.ascontiguousarray(np.asarray(a, dtype=np.float32))
    x = f(x)
    rep = lambda v, n=128: np.ascontiguousarray(np.broadcast_to(f(v).reshape(1, -1), (n, f(v).size)))
    shared = {
        "g1B": rep(norm1_g[0]),
        "g2B": rep(norm2_g[0]),
        "w_in": f(w_in[0]),
        "bgT": np.ascontiguousarray(f(b_gate[0]).reshape(32, 128).T),
        "bfB": rep(np.tile(f(b_forget[0]), 16)),
        "lngB": rep(sgu_ln_g[0]),
        "lnbB": rep(sgu_ln_b[0]),
        "wsT": np.ascontiguousarray(f(w_spatial[0]).transpose(2, 0, 1).reshape(128, 1024)),
        "bspB": rep(f(b_spatial[0]).reshape(-1)),
        "qg": f(q_norm_g[0]).reshape(128, 1),
        "kg": f(k_norm_g[0]).reshape(128, 1),
        "wps": f(w_proj_sgu[0]),
        "wpf": f(w_proj_fox[0]),
        "wout": f(w_out[0]),
        "wr": np.ascontiguousarray(np.concatenate([f(w_router_group[0]), f(w_router_expert[0])], axis=1)),
        "brB": rep(np.concatenate([f(b_router_group[0]), f(b_router_expert[0])])),
        "weg": f(w_expert_gate[0]),
        "weu": f(w_expert_up[0]),
        "wed": f(w_expert_down[0]),
    }
    zeros = np.zeros((1024, 2048), np.float32)
    in_maps = []
    for c in range(8):
        b, half = c // 2, c % 2
        m = dict(shared)
        m["xo"] = np.ascontiguousarray(x[b, half * 1024:(half + 1) * 1024])
        m["xp"] = np.ascontiguousarray(x[b, 0:1024]) if half == 1 else zeros
        m["maskb"] = np.full((128, 1), 0.0 if half == 1 else -30000.0, np.float32)
        in_maps.append(m)
    return in_maps


_NC = {}


def kernel(**inputs):
    in_maps = prep_inputs(**inputs)
    if "nc" not in _NC:
        _NC["nc"] = build()
    res = run_bass_kernel_spmd(_NC["nc"], in_maps, core_ids=list(range(8)))
    out = np.zeros((4, 2048, 2048), np.float32)
    for c in range(8):
        b, half = c // 2, c % 2
        out[b, half * 1024:(half + 1) * 1024] = res.results[c]["out"]
    return out
```
